# Optimizing a Trainium2 kernel written in Bass

```python
import math
import jax, jax.numpy as jnp
from jax import lax
import numpy as np

D_MODEL = 2048
BATCH = 2
SEQ = 4096
DEPTH = 4

CTX_LEN = 256
GRID_W = 64
HEAD_DIM = 128

A_WIDTH = D_MODEL // 4
A_HEADS = A_WIDTH // HEAD_DIM
A_CHUNK = 128

B_WIDTH = D_MODEL // 2
B_QK_DIM = HEAD_DIM
B_V_DIM = 2 * HEAD_DIM
B_HEADS = B_WIDTH // B_V_DIM
B_QK_WIDTH = B_HEADS * 2 * B_QK_DIM
Q_BLOCK = 128
ROPE_THETA = 10000.0

C_WIDTH = D_MODEL // 4
C_VAL_DIM = HEAD_DIM
C_KEY_DIM = HEAD_DIM
C_HEADS = C_WIDTH // C_VAL_DIM
C_KEY_WIDTH = C_HEADS * C_KEY_DIM
SCAN_CHUNK = 64

IN_SPLITS = (A_WIDTH, A_WIDTH,
             B_QK_WIDTH, B_QK_WIDTH, B_WIDTH,
             C_KEY_WIDTH, C_KEY_WIDTH, C_KEY_WIDTH,
             C_WIDTH, C_WIDTH)
IN_WIDTH = sum(IN_SPLITS)
MIX_WIDTH = A_WIDTH + B_WIDTH + C_WIDTH

N_EXPERTS = 64
TOP_K = 8
EXPERT_DIM = 384
ROUTED_SCALE = 2.5
MOE_BLOCK = 128

DEEPNORM_ALPHA = (2.0 * DEPTH) ** 0.25
DEEPNORM_BETA = (8.0 * DEPTH) ** -0.25
LN_EPS = 1e-5
NORM_EPS = 1e-5
F32 = jnp.float32

kernel_name = 'hybrid_gmlp_diffattn_hgrn2_moe_dit'


def layer_norm(x, g, b):
    xf = x.astype(F32)
    mu = jnp.mean(xf, axis=-1, keepdims=True)
    var = jnp.mean(jnp.square(xf - mu), axis=-1, keepdims=True)
    return ((xf - mu) * lax.rsqrt(var + LN_EPS)).astype(x.dtype) * g + b


def rms_norm(x, g):
    xf = x.astype(F32)
    return (xf * lax.rsqrt(jnp.mean(jnp.square(xf), axis=-1, keepdims=True) + NORM_EPS)).astype(x.dtype) * g


def modulate(h, shift, scale):
    return h * (1 + scale) + shift


def axial_rope_tables(n):
    t = jnp.arange(n)
    row = (t // GRID_W).astype(F32)
    col = (t % GRID_W).astype(F32)
    axis_dim = HEAD_DIM // 2
    inv = ROPE_THETA ** (-jnp.arange(0, axis_dim, 2, dtype=F32) / axis_dim)
    ar = row[:, None] * inv
    ac = col[:, None] * inv
    cos = jnp.concatenate([jnp.cos(ar), jnp.cos(ar), jnp.cos(ac), jnp.cos(ac)], axis=-1)
    sin = jnp.concatenate([jnp.sin(ar), jnp.sin(ar), jnp.sin(ac), jnp.sin(ac)], axis=-1)
    return cos, sin


def apply_axial_rope(x, cos, sin):
    x1, x2, x3, x4 = jnp.split(x, 4, axis=-1)
    rot = jnp.concatenate([-x2, x1, -x4, x3], axis=-1)
    c = cos[:, None, None, :]
    s = sin[:, None, None, :]
    return (x.astype(F32) * c + rot.astype(F32) * s).astype(x.dtype)


def chunk_gmlp(u, v, ln_g, ln_b, w_s, b_s):
    bsz, n, _ = u.shape
    u = jax.nn.gelu(u)
    v = jax.nn.gelu(v).reshape(bsz, n // A_CHUNK, A_CHUNK, A_HEADS, HEAD_DIM)
    v = layer_norm(v, ln_g.reshape(A_HEADS, HEAD_DIM), ln_b.reshape(A_HEADS, HEAD_DIM))
    s = jnp.einsum('hpq,bcqhd->bcphd', w_s, v) + b_s.T[:, :, None]
    return u * s.reshape(bsz, n, A_WIDTH)


def diff_attend(q, k, v, lam):
    s = jnp.einsum('bqhmd,bshmd->bhmqs', q, k).astype(F32) * (B_QK_DIM ** -0.5)
    p = jax.nn.softmax(s, axis=-1)
    a = p[:, :, 0] - lam * p[:, :, 1]
    return jnp.einsum('bhqs,bshe->bqhe', a.astype(v.dtype), v)


def diff_attention(q_lat, k_lat, v_lat, q_ctx, k_ctx, v_ctx, lam, lam_init, subln_g, need_ctx):
    bsz, n = q_lat.shape[0], q_lat.shape[1]
    cos, sin = axial_rope_tables(n)
    q_lat = apply_axial_rope(q_lat, cos, sin)
    k_lat = apply_axial_rope(k_lat, cos, sin)
    k_all = jnp.concatenate([k_ctx, k_lat], axis=1)
    v_all = jnp.concatenate([v_ctx, v_lat], axis=1)
    nb = n // Q_BLOCK
    qb = q_lat.reshape(bsz, nb, Q_BLOCK, B_HEADS, 2, B_QK_DIM).swapaxes(0, 1)
    o = lax.map(lambda blk: diff_attend(blk, k_all, v_all, lam), qb)
    o = o.swapaxes(0, 1).reshape(bsz, n, B_HEADS, B_V_DIM)

    def post(t):
        return (rms_norm(t, subln_g) * (1.0 - lam_init)).reshape(t.shape[0], t.shape[1], B_WIDTH)

    out_lat = post(o)
    out_ctx = post(diff_attend(q_ctx, k_ctx, v_ctx, lam)) if need_ctx else None
    return out_lat, out_ctx


def gla_chunk_scan(q, k, v, logf, s0):
    bsz, h, n, _ = q.shape
    dv = v.shape[-1]
    nc = n // SCAN_CHUNK

    def to_chunks(t):
        return jnp.moveaxis(t.astype(F32).reshape(bsz, h, nc, SCAN_CHUNK, t.shape[-1]), 2, 0)

    mask = jnp.tril(jnp.ones((SCAN_CHUNK, SCAN_CHUNK), dtype=bool))[:, :, None]

    def step(S, inp):
        qc, kc, vc, gc = inp
        G = jnp.cumsum(gc, axis=-2)
        diff = G[..., :, None, :] - G[..., None, :, :]
        decay = jnp.exp(jnp.where(mask, diff, -jnp.inf))
        A = jnp.einsum('bhtd,bhsd,bhtsd->bhts', qc, kc, decay)
        o = jnp.einsum('bhts,bhse->bhte', A, vc) + jnp.einsum('bhtd,bhde->bhte', qc * jnp.exp(G), S)
        G_end = G[..., -1:, :]
        S = jnp.exp(G_end)[..., 0, :, None] * S + jnp.einsum('bhsd,bhse->bhde', kc * jnp.exp(G_end - G), vc)
        return S, o

    S, o = lax.scan(step, s0.astype(F32), (to_chunks(q), to_chunks(k), to_chunks(v), to_chunks(logf)))
    o = jnp.moveaxis(o, 0, 2).reshape(bsz, h, n, dv).astype(q.dtype)
    return o, S


def gla_final_state(k, v, logf, s0):
    G = jnp.cumsum(logf.astype(F32), axis=2)
    G_end = G[:, :, -1:, :]
    return (jnp.exp(G_end)[:, :, 0, :, None] * s0
            + jnp.einsum('bhsd,bhse->bhde', k.astype(F32) * jnp.exp(G_end - G), v.astype(F32)))


def _heads(t, h, d):
    bsz, n, _ = t.shape
    return t.reshape(bsz, n, h, d).transpose(0, 2, 1, 3)


def hgrn_gates(f_raw, lb):
    lb = lb.astype(F32).reshape(C_HEADS, 1, C_KEY_DIM)
    f = lb + (1.0 - lb) * jax.nn.sigmoid(_heads(f_raw, C_HEADS, C_KEY_DIM).astype(F32))
    return jnp.log(f), 1.0 - f


def hgrn2_mixer(lat, ctxp, lb_f, lb_b, norm_g, need_ctx):
    q_l, ff_l, fb_l, i_l, g_l = lat
    q_c, ff_c, fb_c, i_c, g_c = ctxp
    bsz = q_l.shape[0]
    s0 = jnp.zeros((bsz, C_HEADS, C_KEY_DIM, C_VAL_DIM), F32)
    flip = lambda t: jnp.flip(t, axis=2)

    def readout(o, gate):
        n = o.shape[2]
        o = rms_norm(o.transpose(0, 2, 1, 3), norm_g)
        return (o * jax.nn.silu(gate.reshape(bsz, n, C_HEADS, C_VAL_DIM))).reshape(bsz, n, C_WIDTH)

    ci = _heads(i_c, C_HEADS, C_VAL_DIM)
    clf, ckf = hgrn_gates(ff_c, lb_f)
    clb, ckb = hgrn_gates(fb_c, lb_b)
    if need_ctx:
        cq = _heads(jax.nn.silu(q_c), C_HEADS, C_KEY_DIM)
        o_cf, S_f = gla_chunk_scan(cq, ckf, ci, clf, s0)
        o_cb, S_b = gla_chunk_scan(flip(cq), flip(ckb), flip(ci), flip(clb), s0)
        out_ctx = readout(o_cf + flip(o_cb), g_c)
    else:
        S_f = gla_final_state(ckf, ci, clf, s0)
        S_b = gla_final_state(flip(ckb), flip(ci), flip(clb), s0)
        out_ctx = None

    lq = _heads(jax.nn.silu(q_l), C_HEADS, C_KEY_DIM)
    li = _heads(i_l, C_HEADS, C_VAL_DIM)
    llf, lkf = hgrn_gates(ff_l, lb_f)
    llb, lkb = hgrn_gates(fb_l, lb_b)
    o_f, _ = gla_chunk_scan(lq, lkf, li, llf, S_f)
    o_b, _ = gla_chunk_scan(flip(lq), flip(lkb), flip(li), flip(llb), S_b)
    out_lat = readout(o_f + flip(o_b), g_l)
    return out_lat, out_ctx


def moe_ffn(h, router_w, router_b, w_gate, w_up, w_down, s_gate, s_up, s_down):
    T = h.shape[0]
    scores = jax.nn.sigmoid((h @ router_w).astype(F32))
    _, idx = lax.top_k(scores + router_b.astype(F32), TOP_K)
    w = jnp.take_along_axis(scores, idx, axis=-1)
    w = w / jnp.sum(w, axis=-1, keepdims=True) * ROUTED_SCALE
    combine = jnp.sum(jax.nn.one_hot(idx, N_EXPERTS, dtype=F32) * w[..., None], axis=1).astype(h.dtype)
    nblk = T // MOE_BLOCK

    def block(args):
        hb, cb = args
        a = jax.nn.silu(jnp.einsum('td,edf->tef', hb, w_gate)) * jnp.einsum('td,edf->tef', hb, w_up)
        return jnp.einsum('tef,efd->td', a * cb[..., None], w_down)

    routed = lax.map(block, (h.reshape(nblk, MOE_BLOCK, -1),
                             combine.reshape(nblk, MOE_BLOCK, N_EXPERTS))).reshape(T, -1)
    shared = (jax.nn.silu(h @ s_gate) * (h @ s_up)) @ s_down
    return routed + shared


def setup_inputs(seed: int = 0) -> dict:
    key = jax.random.key(seed)
    ks = iter(jax.random.split(key, 32))
    D, E, F = D_MODEL, N_EXPERTS, EXPERT_DIM

    def nrm(shape, scale):
        return jax.random.normal(next(ks), shape, jnp.float32) * scale

    return {
        'x': nrm((BATCH, SEQ, D), 1.0),
        'c': nrm((BATCH, D), 1.0),
        'ctx': nrm((BATCH, CTX_LEN, D), 1.0),
        'c_ctx': nrm((D,), 1.0),
        'w_mod': nrm((DEPTH, D, 6 * D), 0.5 * D ** -0.5),
        'b_mod': nrm((DEPTH, 6 * D), 0.01),
        'w_in': nrm((DEPTH, D, IN_WIDTH), D ** -0.5),
        'w_out': nrm((DEPTH, MIX_WIDTH, D), DEEPNORM_BETA * MIX_WIDTH ** -0.5),
        'gmlp_ln_g': 1.0 + nrm((DEPTH, A_WIDTH), 0.01),
        'gmlp_ln_b': nrm((DEPTH, A_WIDTH), 0.01),
        'gmlp_ws': nrm((DEPTH, A_HEADS, A_CHUNK, A_CHUNK), A_CHUNK ** -0.5),
        'gmlp_bs': 1.0 + nrm((DEPTH, A_HEADS, A_CHUNK), 0.01),
        'diff_lam': nrm((DEPTH, 4, B_QK_DIM), 0.1),
        'diff_subln_g': 1.0 + nrm((DEPTH, B_V_DIM), 0.01),
        'hgrn_lb': nrm((DEPTH, 2, C_KEY_WIDTH), 0.5),
        'hgrn_norm_g': 1.0 + nrm((DEPTH, C_VAL_DIM), 0.01),
        'ln1_g': 1.0 + nrm((DEPTH, D), 0.01),
        'ln1_b': nrm((DEPTH, D), 0.01),
        'ln2_g': 1.0 + nrm((DEPTH, D), 0.01),
        'ln2_b': nrm((DEPTH, D), 0.01),
        'router_w': nrm((DEPTH, D, E), D ** -0.5),
        'router_b': nrm((DEPTH, E), 0.01),
        'exp_w_gate': nrm((DEPTH, E, D, F), D ** -0.5),
        'exp_w_up': nrm((DEPTH, E, D, F), D ** -0.5),
        'exp_w_down': nrm((DEPTH, E, F, D), DEEPNORM_BETA * F ** -0.5),
        'sh_w_gate': nrm((DEPTH, D, F), D ** -0.5),
        'sh_w_up': nrm((DEPTH, D, F), D ** -0.5),
        'sh_w_down': nrm((DEPTH, F, D), DEEPNORM_BETA * F ** -0.5),
    }


def reference(x, c, ctx, c_ctx, w_mod, b_mod, w_in, w_out, gmlp_ln_g, gmlp_ln_b, gmlp_ws, gmlp_bs,
              diff_lam, diff_subln_g, hgrn_lb, hgrn_norm_g, ln1_g, ln1_b, ln2_g, ln2_b,
              router_w, router_b, exp_w_gate, exp_w_up, exp_w_down, sh_w_gate, sh_w_up, sh_w_down):
    bsz, n, D = x.shape
    L = ctx.shape[1]
    split_idx = [int(v) for v in np.cumsum(IN_SPLITS)[:-1]]
    sm = jax.nn.softmax(hgrn_lb.astype(F32), axis=0)
    lb_all = jnp.cumsum(sm, axis=0) - sm[0]
    silu_c = jax.nn.silu(c)
    silu_cc = jax.nn.silu(c_ctx)
    cx = ctx
    for l in range(DEPTH):
        need_ctx = l < DEPTH - 1
        mod = silu_c @ w_mod[l] + b_mod[l]
        modc = silu_cc @ w_mod[l] + b_mod[l]
        sh1, sc1, g1, sh2, sc2, g2 = jnp.split(mod[:, None, :], 6, axis=-1)
        sh1c, sc1c, g1c, sh2c, sc2c, g2c = jnp.split(modc, 6)

        px = jnp.split(modulate(x, sh1, sc1) @ w_in[l], split_idx, axis=-1)
        pc = jnp.split(modulate(cx, sh1c, sc1c) @ w_in[l], split_idx, axis=-1)
        au, av, bq, bk, bv, cq, cff, cfb, ci, cg = px
        au_c, av_c, bq_c, bk_c, bv_c, cq_c, cff_c, cfb_c, ci_c, cg_c = pc

        a_x = chunk_gmlp(au, av, gmlp_ln_g[l], gmlp_ln_b[l], gmlp_ws[l], gmlp_bs[l])

        lam_init = 0.8 - 0.6 * math.exp(-0.3 * l)
        dl = diff_lam[l].astype(F32)
        lam = jnp.exp(jnp.sum(dl[0] * dl[1])) - jnp.exp(jnp.sum(dl[2] * dl[3])) + lam_init
        qk_shape = (B_HEADS, 2, B_QK_DIM)
        b_x, b_c = diff_attention(
            bq.reshape(bsz, n, *qk_shape), bk.reshape(bsz, n, *qk_shape), bv.reshape(bsz, n, B_HEADS, B_V_DIM),
            bq_c.reshape(bsz, L, *qk_shape), bk_c.reshape(bsz, L, *qk_shape), bv_c.reshape(bsz, L, B_HEADS, B_V_DIM),
            lam, lam_init, diff_subln_g[l], need_ctx)

        c_x, c_c = hgrn2_mixer((cq, cff, cfb, ci, cg), (cq_c, cff_c, cfb_c, ci_c, cg_c),
                               lb_all[l, 0], lb_all[l, 1], hgrn_norm_g[l], need_ctx)

        mix_x = jnp.concatenate([a_x, b_x, c_x], axis=-1) @ w_out[l]
        x = layer_norm(DEEPNORM_ALPHA * x + g1 * mix_x, ln1_g[l], ln1_b[l])

        hx = modulate(x, sh2, sc2)
        moe_args = (router_w[l], router_b[l], exp_w_gate[l], exp_w_up[l], exp_w_down[l],
                    sh_w_gate[l], sh_w_up[l], sh_w_down[l])
        if need_ctx:
            a_c = chunk_gmlp(au_c, av_c, gmlp_ln_g[l], gmlp_ln_b[l], gmlp_ws[l], gmlp_bs[l])
            mix_c = jnp.concatenate([a_c, b_c, c_c], axis=-1) @ w_out[l]
            cx = layer_norm(DEEPNORM_ALPHA * cx + g1c * mix_c, ln1_g[l], ln1_b[l])
            hc = modulate(cx, sh2c, sc2c)
            y = moe_ffn(jnp.concatenate([hc, hx], axis=1).reshape(-1, D), *moe_args).reshape(bsz, L + n, D)
            y_c, y_x = y[:, :L], y[:, L:]
            cx = layer_norm(DEEPNORM_ALPHA * cx + g2c * y_c, ln2_g[l], ln2_b[l])
        else:
            y_x = moe_ffn(hx.reshape(-1, D), *moe_args).reshape(bsz, n, D)
        x = layer_norm(DEEPNORM_ALPHA * x + g2 * y_x, ln2_g[l], ln2_b[l])
    return x
```

```python
import numpy as np
from contextlib import ExitStack, contextmanager
import concourse.bass as bass
import concourse.mybir as mybir
from concourse.bass_utils import run_bass_kernel_spmd

F32 = mybir.dt.float32
BF16 = mybir.dt.bfloat16
AF = mybir.ActivationFunctionType
ALU = mybir.AluOpType
AX = mybir.AxisListType

NSLOT = 12


class Prog:
    ENG = ("pe", "dve", "act", "pool", "sp")
    DQ = ("sp", "pool")

    def __init__(self, nc):
        self.nc = nc
        self.ops = []
        self.es = ExitStack()
        self.uid = 0

    def sb(self, name, shape, dt=F32, es=None):
        self.uid += 1
        return (es or self.es).enter_context(self.nc.sbuf_tensor("%s_%d" % (name, self.uid), list(shape), dt))

    def ps(self, name, shape, dt=F32, es=None):
        self.uid += 1
        return (es or self.es).enter_context(self.nc.psum_tensor("%s_%d" % (name, self.uid), list(shape), dt))

    def dram(self, name, shape, dt, kind):
        return self.nc.dram_tensor(name, list(shape), dt, kind=kind).ap()

    @contextmanager
    def scope(self):
        es = ExitStack()
        try:
            yield es
        finally:
            self.barrier()
            es.close()

    def op(self, eng, fn, r=(), w=(), dma=False):
        self.ops.append(dict(eng=eng, fn=fn, r=tuple(r), w=tuple(w), dma=dma))

    def barrier(self):
        self.ops.append(dict(eng="barrier", fn=None, r=(), w=(), dma=False))

    def eng_obj(self, e):
        nc = self.nc
        return dict(pe=nc.tensor, dve=nc.vector, act=nc.scalar, pool=nc.gpsimd, sp=nc.sync)[e]

    def emit(self):
        nc = self.nc
        ops = self.ops
        n = len(ops)
        last_w = {}
        readers = {}
        deps = [None] * n
        needed = [False] * n
        last_on = {e: None for e in self.ENG}
        for i, o in enumerate(ops):
            if o["eng"] == "barrier":
                last_w = {}
                readers = {}
                for e in self.ENG:
                    if last_on[e] is not None:
                        needed[last_on[e]] = True
                deps[i] = set()
                continue
            d = set()
            for k in o["r"]:
                if k in last_w:
                    d.add(last_w[k])
            for k in o["w"]:
                if k in last_w:
                    d.add(last_w[k])
                for rr in readers.get(k, ()):
                    d.add(rr)
            d.discard(i)
            if o["eng"] == "pe":
                d = {j for j in d if not (ops[j]["eng"] == "pe" and not ops[j]["dma"])}
            deps[i] = d
            for k in o["r"]:
                readers.setdefault(k, []).append(i)
            for k in o["w"]:
                last_w[k] = i
                readers[k] = []
            if not o["dma"]:
                last_on[o["eng"]] = i
        for i in range(n):
            for j in deps[i]:
                needed[j] = True
        if True:
            for e in self.ENG:
                if last_on[e] is not None:
                    needed[last_on[e]] = True
        csem = {e: self.es.enter_context(nc.semaphore("c_" + e)) for e in self.ENG}
        dsem = {e: [self.es.enter_context(nc.semaphore("d_%s_%d" % (e, s))) for s in range(NSLOT)]
                for e in self.DQ}
        ccount = {e: 0 for e in self.ENG}
        dcount = {e: 0 for e in self.DQ}
        sig = [None] * n
        waited = {e: {} for e in self.ENG}

        def wait(e, sem, val):
            key = id(sem)
            if waited[e].get(key, 0) >= val:
                return
            self.eng_obj(e).wait_ge(sem, val)
            waited[e][key] = val

        def wait_all(e):
            for e2 in self.ENG:
                if ccount[e2] > 0:
                    wait(e, csem[e2], ccount[e2])
            for q in self.DQ:
                for s in range(NSLOT):
                    cnt = (dcount[q] - s + NSLOT - 1) // NSLOT if dcount[q] > s else 0
                    if cnt > 0:
                        wait(e, dsem[q][s], 16 * cnt)

        for i, o in enumerate(ops):
            e = o["eng"]
            if e == "barrier":
                for e2 in self.ENG:
                    wait_all(e2)
                continue
            eo = self.eng_obj(e)
            for j in sorted(deps[i]):
                s, v = sig[j]
                wait(e, s, v)
            if o["dma"]:
                jn = dcount[e]
                dcount[e] += 1
                sem = dsem[e][jn % NSLOT]
                prev = 16 * (jn // NSLOT)
                if prev > 0:
                    wait(e, sem, prev)
                ins = o["fn"](eo)
                ins.then_inc(sem, 16)
                sig[i] = (sem, prev + 16)
            else:
                ins = o["fn"](eo)
                if needed[i]:
                    ccount[e] += 1
                    ins.then_inc(csem[e], 1)
                    sig[i] = (csem[e], ccount[e])
        wait_all("sp")
        self.stats = dict(n_ops=n, ccount=dict(ccount), dcount=dict(dcount))
        self.es.close()


D = 2048
KC = 16
L_CTX = 256
N_LAT = 4096
NT = L_CTX + N_LAT
TILES = [(0, 256, 1)] + [(256 + 512 * i, 512, 0) for i in range(8)]
LN_EPS = 1e-5
CH = 32


def DMA(P, q, out, in_, r=(), w=()):
    P.op(q, lambda e, o=out, i=in_: e.dma_start(out=o, in_=i), r=r, w=w, dma=True)


def MM(P, out, lhsT, rhs, start, stop, r=(), w=()):
    P.op("pe", lambda e, o=out, a=lhsT, b=rhs, s=start, t=stop: e.matmul(o, a, b, start=s, stop=t), r=r, w=w)


def TR(P, out, in_, ident, r=(), w=()):
    P.op("pe", lambda e, o=out, a=in_, b=ident: e.transpose(o, a, b), r=r, w=w)


def ACT(P, out, in_, func, r=(), w=(), **kw):
    P.op("act", lambda e, o=out, i=in_, f=func, k=kw: e.activation(out=o, in_=i, func=f, **k), r=r, w=w)


def TT(P, eng, out, in0, in1, op, r=(), w=()):
    P.op(eng, lambda e, o=out, a=in0, b=in1, p=op: e.tensor_tensor(out=o, in0=a, in1=b, op=p), r=r, w=w)


def TS(P, eng, out, in0, s1, s2, op0, op1, r=(), w=()):
    if s2 is None:
        P.op(eng, lambda e, o=out, a=in0, x=s1, p0=op0: e.tensor_scalar(out=o, in0=a, scalar1=x, scalar2=None, op0=p0), r=r, w=w)
    else:
        P.op(eng, lambda e, o=out, a=in0, x=s1, y=s2, p0=op0, p1=op1: e.tensor_scalar(out=o, in0=a, scalar1=x, scalar2=y, op0=p0, op1=p1), r=r, w=w)


def STT(P, eng, out, in0, scalar, in1, op0, op1, r=(), w=()):
    P.op(eng, lambda e, o=out, a=in0, s=scalar, b=in1, p0=op0, p1=op1: e.scalar_tensor_tensor(out=o, in0=a, scalar=s, in1=b, op0=p0, op1=p1), r=r, w=w)


def CP(P, eng, out, in_, r=(), w=()):
    if eng == "act":
        P.op("act", lambda e, o=out, i=in_: e.copy(out=o, in_=i), r=r, w=w)
    else:
        P.op(eng, lambda e, o=out, i=in_: e.tensor_copy(out=o, in_=i), r=r, w=w)


def RSQRT(P, out, in_, scale, eps, r=(), w=()):
    ACT(P, out, in_, AF.Sqrt, r=r, w=w, scale=scale, bias=eps)
    P.op("dve", lambda e, o=out: e.reciprocal(out=o, in_=o), r=w, w=w)


def mod_vectors(P, es, pbank, pkey, wm, bm, cin, ncolchunks, name):
    mod = P.sb(name, [128, 2, ncolchunks])
    cs = P.sb("cs", [128, KC, 2], es=es)
    bms = P.sb("bms", [128, ncolchunks], es=es)
    DMA(P, "sp", cs[:], cin, w=["cs"])
    DMA(P, "sp", bms[:], bm, w=["bms"])
    ACT(P, cs[:], cs[:], AF.Silu, r=["cs"], w=["cs"])
    wbuf = [P.sb("wmb", [128, KC, 512], es=es) for _ in range(2)]
    wmv = wm.rearrange("(k p) c -> p k c", p=128)
    nblk = ncolchunks // 4
    for blk in range(nblk):
        wb = wbuf[blk % 2]
        for kh in range(2):
            DMA(P, "sp", wb[:, kh * 8:(kh + 1) * 8, :], wmv[:, kh * 8:(kh + 1) * 8, blk * 512:(blk + 1) * 512], w=[("wmb", blk % 2)])
        for jj in range(4):
            j = blk * 4 + jj
            for k in range(KC):
                MM(P, pbank[:, 2 * j:2 * j + 2], wb[:, k, jj * 128:(jj + 1) * 128], cs[:, k, :], k == 0, k == KC - 1,
                   r=[("wmb", blk % 2), "cs"], w=[pkey])
    for n in range(2):
        TT(P, "dve", mod[:, n, :], pbank[:, n:2 * ncolchunks:2], bms[:], ALU.add, r=[pkey, "bms"], w=[name])
    return mod


def build_M():
    nc = bass.Bass("TRN2", target_bir_lowering=False)
    P = Prog(nc)
    I = lambda n, s, dt=F32: P.dram(n, s, dt, "ExternalInput")
    xT = I("xT", [D, NT])
    modin = I("modin", [128, 2, 32])
    win = I("win", [D, 1664])
    lng = I("lng", [1, 128]); lnb = I("lnb", [1, 128]); wsT = I("wsT", [128, 128]); bsr = I("bsr", [1, 128])
    dlam = I("dlam", [1, 512]); sgl = I("sgl", [128, 2]); lami = I("lami", [128, 2])
    lbraw = I("lbraw", [128, 2, 4]); wsel = I("wsel", [128, 2, 4]); normg = I("normg", [128, 1])
    identd = I("identd", [128, 128]); rmd = I("rmd", [128, 128])
    cosd = I("cosd", [128, N_LAT]); sind = I("sind", [128, N_LAT])
    mask32d = I("mask32d", [128, 512]); trid = I("trid", [CH, 1024])
    mixT = P.dram("mixT", [512, NT], BF16, "ExternalOutput")
    yT = P.dram("yT_s", [9, 128, NT], F32, "Internal")
    ytok = P.dram("ytok_s", [NT, 512], F32, "Internal")

    pb = [P.ps("pb", [128, 512]) for _ in range(8)]
    PK = lambda i: ("pb", i)
    ident = P.sb("ident", [128, 128]); ones32 = P.sb("ones32", [128, 128]); onesb = P.sb("onesb", [128, 128], BF16)
    DMA(P, "sp", ident[:], identd, w=["ident"])
    P.op("dve", lambda e: e.memset(ones32[:], 1.0), w=["ones32"])
    P.op("dve", lambda e: e.memset(onesb[:], 1.0), w=["onesb"])

    mod1 = P.sb("mod1", [128, 2, 32])
    DMA(P, "sp", mod1[:], modin, w=["mod1"])
    for n in range(2):
        TS(P, "dve", mod1[:, n, 16:32], mod1[:, n, 16:32], 1.0, None, ALU.add, None, r=["mod1"], w=["mod1"])

    with P.scope() as es:
        wi = P.sb("wi", [128, KC, 1664], BF16, es=es)
        winv = win.rearrange("(k p) c -> p k c", p=128)
        for k4 in range(4):
            DMA(P, "pool", wi[:, k4 * 4:(k4 + 1) * 4, :], winv[:, k4 * 4:(k4 + 1) * 4, :], w=["wi"])
        stage = [P.sb("xst", [128, 512], es=es) for _ in range(4)]
        xm = [P.sb("xm", [128, KC, 512], BF16, es=es) for _ in range(2)]
        ost = [P.sb("ost", [128, 512], es=es) for _ in range(4)]
        fm_func = [AF.Gelu, AF.Copy, AF.Copy, AF.Copy, AF.Copy, AF.Silu, AF.Sigmoid, AF.Sigmoid, AF.Silu]
        si = 0
        oi = 0
        pi = 0
        def load_tile(ti):
            nonlocal si
            t0, N, n = TILES[ti]
            xmt = xm[ti % 2]
            xk = ("xm", ti % 2)
            for k in range(KC):
                st = stage[si % 4]; sk = ("xst", si % 4); si += 1
                DMA(P, "sp", st[:, :N], xT[k * 128:(k + 1) * 128, t0:t0 + N], w=[sk])
                TS(P, "dve", xmt[:, k, :N], st[:, :N], mod1[:, n, 16 + k:17 + k], mod1[:, n, k:k + 1],
                   ALU.mult, ALU.add, r=[sk, "mod1"], w=[xk])

        load_tile(0)
        for ti, (t0, N, n) in enumerate(TILES):
            xmt = xm[ti % 2]
            xk = ("xm", ti % 2)
            if ti + 1 < len(TILES):
                load_tile(ti + 1)
            for c in range(9):
                pbk = pb[pi % 4]; pk = PK(pi % 4); pi += 1
                for k in range(KC):
                    MM(P, pbk[:, :N], wi[:, k, c * 128:(c + 1) * 128], xmt[:, k, :N], k == 0, k == KC - 1, r=["wi", xk], w=[pk])
                o = ost[oi % 4]; ok = ("ost", oi % 4); oi += 1
                ACT(P, o[:, :N], pbk[:, :N], fm_func[c], r=[pk], w=[ok])
                DMA(P, "pool", yT[c, :, t0:t0 + N], o[:, :N], r=[ok], w=[("yT", c, ti)])
            for j in range(N // 128):
                pbk = pb[pi % 4]; pk = PK(pi % 4); pi += 1
                for k in range(KC):
                    MM(P, pbk[:, :], xmt[:, k, j * 128:(j + 1) * 128], wi[:, k, 1152:1664], k == 0, k == KC - 1, r=["wi", xk], w=[pk])
                o = ost[oi % 4]; ok = ("ost", oi % 4); oi += 1
                ACT(P, o[:, 0:128], pbk[:, 0:128], AF.Gelu, r=[pk], w=[ok])
                CP(P, "dve", o[:, 128:512], pbk[:, 128:512], r=[pk], w=[ok])
                DMA(P, "pool", ytok[t0 + j * 128:t0 + (j + 1) * 128, :], o[:, :], r=[ok], w=[("ytok", (t0 // 128) + j)])

    with P.scope() as es:
        lng_b = P.sb("lng_b", [128, 128], es=es); lnb_b = P.sb("lnb_b", [128, 128], es=es); bs_b = P.sb("bs_b", [128, 128], es=es)
        wsb = P.sb("wsb", [128, 128], BF16, es=es)
        abuf = P.sb("abuf", [128, NT], BF16, es=es)
        DMA(P, "sp", lng_b[:], lng.broadcast_to([128, 128]), w=["lng_b"])
        DMA(P, "sp", lnb_b[:], lnb.broadcast_to([128, 128]), w=["lnb_b"])
        DMA(P, "sp", bs_b[:], bsr.broadcast_to([128, 128]), w=["bs_b"])
        DMA(P, "pool", wsb[:], wsT, w=["wsb"])
        NB = 3
        vt = [P.sb("g_v", [128, 128], es=es) for _ in range(NB)]
        ut = [P.sb("g_u", [128, 128], es=es) for _ in range(NB)]
        stt = [P.sb("g_st", [128, 6], es=es) for _ in range(NB)]
        mv = [P.sb("g_mv", [128, 2], es=es) for _ in range(NB)]
        vn = [P.sb("g_vn", [128, 128], es=es) for _ in range(NB)]
        vl = [P.sb("g_vl", [128, 128], BF16, es=es) for _ in range(NB)]
        sst = [P.sb("g_s", [128, 128], es=es) for _ in range(NB)]
        for ch in range(NT // 128):
            b = ch % NB
            t0 = ch * 128
            K = lambda s: (s, b)
            DMA(P, "sp", vt[b][:], ytok[t0:t0 + 128, 0:128], w=[K("v")])
            DMA(P, "sp", ut[b][:], yT[0, :, t0:t0 + 128], w=[K("u")])
            P.op("dve", lambda e, o=stt[b], i=vt[b]: e.bn_stats(out=o[:], in_=i[:]), r=[K("v")], w=[K("st")])
            P.op("dve", lambda e, o=mv[b], i=stt[b]: e.bn_aggr(out=o[:], in_=i[:]), r=[K("st")], w=[K("mv")])
            RSQRT(P, mv[b][:, 1:2], mv[b][:, 1:2], 1.0, LN_EPS, r=[K("mv")], w=[K("mv")])
            TS(P, "dve", vn[b][:], vt[b][:], mv[b][:, 0:1], mv[b][:, 1:2], ALU.subtract, ALU.mult, r=[K("v"), K("mv")], w=[K("vn")])
            TT(P, "pool", vn[b][:], vn[b][:], lng_b[:], ALU.mult, r=[K("vn"), "lng_b"], w=[K("vn")])
            TT(P, "pool", vl[b][:], vn[b][:], lnb_b[:], ALU.add, r=[K("vn"), "lnb_b"], w=[K("vl")])
            pk = PK(b)
            MM(P, pb[b][:, 0:128], vl[b][:], wsb[:], True, True, r=[K("vl"), "wsb"], w=[pk])
            TT(P, "dve", sst[b][:], pb[b][:, 0:128], bs_b[:], ALU.add, r=[pk, "bs_b"], w=[K("s")])
            TT(P, "pool", abuf[:, t0:t0 + 128], sst[b][:], ut[b][:], ALU.mult, r=[K("s"), K("u")], w=[("abuf", ch)])
        DMA(P, "sp", mixT[0:128, :], abuf[:], r=[("abuf", ch) for ch in range(NT // 128)])

    with P.scope() as es:
        lbr = P.sb("lbr", [128, 2, 4], es=es); wsl = P.sb("wsl", [128, 2, 4], es=es)
        lbv = P.sb("lbv", [128, 2], es=es); oml = P.sb("oml", [128, 2], es=es); den = P.sb("den", [128, 2], es=es)
        ng = P.sb("ng", [128, 1], es=es)
        m32 = P.sb("m32", [128, 512], es=es); tri = P.sb("tri", [CH, 1024], es=es)
        DMA(P, "sp", lbr[:], lbraw, w=["lbr"]); DMA(P, "sp", wsl[:], wsel, w=["wsl"]); DMA(P, "sp", ng[:], normg, w=["ng"])
        DMA(P, "sp", m32[:], mask32d, w=["m32"]); DMA(P, "sp", tri[:], trid, w=["tri"])
        ACT(P, lbr[:], lbr[:], AF.Exp, r=["lbr"], w=["lbr"])
        P.op("dve", lambda e: e.tensor_reduce(out=den[:], in_=lbr[:], axis=AX.X, op=ALU.add), r=["lbr"], w=["den"])
        P.op("dve", lambda e: e.reciprocal(out=den[:], in_=den[:]), r=["den"], w=["den"])
        TT(P, "dve", lbr[:], lbr[:], wsl[:], ALU.mult, r=["lbr", "wsl"], w=["lbr"])
        P.op("dve", lambda e: e.tensor_reduce(out=lbv[:], in_=lbr[:], axis=AX.X, op=ALU.add), r=["lbr"], w=["lbv"])
        TT(P, "dve", lbv[:], lbv[:], den[:], ALU.mult, r=["lbv", "den"], w=["lbv"])
        TS(P, "dve", oml[:], lbv[:], -1.0, 1.0, ALU.mult, ALU.add, r=["lbv"], w=["oml"])

        ofw = P.sb("ofw", [128, NT], es=es)
        cbuf = P.sb("cbuf", [128, NT], BF16, es=es)
        S32 = P.sb("S32", [128, 128], es=es)
        Sb = [P.sb("Sb", [128, 128], BF16, es=es) for _ in range(2)]
        NB = 2
        sig = [P.sb("h_sig", [128, 512], es=es) for _ in range(NB)]
        qin = [P.sb("h_q", [128, 512], es=es) for _ in range(NB)]
        v32 = [P.sb("h_v", [CH, 16, 128], BF16, es=es) for _ in range(NB)]
        ff = [P.sb("h_f", [128, 512], es=es) for _ in range(NB)]
        lf = [P.sb("h_lf", [128, 512], es=es) for _ in range(NB)]
        kk = [P.sb("h_kk", [128, 512], es=es) for _ in range(NB)]
        G = [P.sb("h_G", [128, 512], es=es) for _ in range(NB)]
        eG = [P.sb("h_eG", [128, 512], es=es) for _ in range(NB)]
        eGn = [P.sb("h_eGn", [128, 512], es=es) for _ in range(NB)]
        qg = [P.sb("h_qg", [128, 512], BF16, es=es) for _ in range(NB)]
        kt = [P.sb("h_kt", [128, 512], es=es) for _ in range(NB)]
        ktb = [P.sb("h_ktb", [128, 512], BF16, es=es) for _ in range(NB)]
        kg = [P.sb("h_kg", [128, 512], es=es) for _ in range(NB)]
        kgt = [P.sb("h_kgt", [CH, 16, 128], BF16, es=es) for _ in range(NB)]
        atm = [P.sb("h_atm", [CH, 512], BF16, es=es) for _ in range(NB)]
        osum = [P.sb("h_os", [128, 512], es=es) for _ in range(NB)]
        osq = [P.sb("h_sq", [128, 512], es=es) for _ in range(NB)]
        rst = [P.sb("h_rs", [128, 512], es=es) for _ in range(NB)]
        sgt = [P.sb("h_sg", [128, 512], es=es) for _ in range(NB)]
        it = 0
        sbi = 0
        pdi = 0
        for d in range(2):
            P.op("dve", lambda e: e.memset(S32[:], 0.0), r=["S32"], w=["S32"])
            P.op("dve", lambda e, s=Sb[sbi % 2]: e.memset(s[:], 0.0), r=[("Sb", sbi % 2)], w=[("Sb", sbi % 2)])
            order = list(range(9)) if d == 0 else [0] + list(range(8, 0, -1))
            rv = lambda ap: ap
            for ti in order:
                t0, N, n = TILES[ti]
                nch = N // CH
                b = it % NB; it += 1
                K = lambda s: (s, b)
                DMA(P, "sp", sig[b][:, :N], yT[6 + d, :, t0:t0 + N], w=[K("sig")])
                DMA(P, "sp", qin[b][:, :N], yT[5, :, t0:t0 + N], w=[K("q")])
                vsrc = ytok[t0:t0 + N, 384:512]
                DMA(P, "pool", v32[b][:, :nch, :], vsrc.rearrange("(c s) e -> s c e", s=CH), w=[K("v32")])
                TS(P, "dve", ff[b][:, :N], rv(sig[b][:, :N]), oml[:, d:d + 1], lbv[:, d:d + 1], ALU.mult, ALU.add, r=[K("sig"), "oml", "lbv"], w=[K("f")])
                ACT(P, lf[b][:, :N], ff[b][:, :N], AF.Ln, r=[K("f")], w=[K("lf")])
                TS(P, "pool", kk[b][:, :N], ff[b][:, :N], -1.0, 1.0, ALU.mult, ALU.add, r=[K("f")], w=[K("kk")])
                if d == 0:
                    P.op("dve", lambda e, o=G[b], m=m32, x=lf[b], N=N: e.tensor_tensor_scan(out=o[:, :N], data0=m[:, :N], data1=x[:, :N], initial=0.0, op0=ALU.mult, op1=ALU.add),
                         r=[K("lf"), "m32"], w=[K("G")])
                else:
                    P.op("dve", lambda e, o=G[b], m=m32, x=lf[b], N=N: e.tensor_tensor_scan(out=o[:, :N][:, ::-1], data0=m[:, :N], data1=x[:, :N][:, ::-1], initial=0.0, op0=ALU.mult, op1=ALU.add),
                         r=[K("lf"), "m32"], w=[K("G")])
                ce = CH - 1 if d == 0 else 0
                ACT(P, eG[b][:, :N], G[b][:, :N], AF.Exp, r=[K("G")], w=[K("eG")])
                ACT(P, eGn[b][:, :N], G[b][:, :N], AF.Exp, r=[K("G")], w=[K("eGn")], scale=-1.0)
                TT(P, "dve", qg[b][:, :N], rv(qin[b][:, :N]), eG[b][:, :N], ALU.mult, r=[K("q"), K("eG")], w=[K("qg")])
                TT(P, "pool", kt[b][:, :N], kk[b][:, :N], eGn[b][:, :N], ALU.mult, r=[K("kk"), K("eGn")], w=[K("kt")])
                CP(P, "pool", ktb[b][:, :N], kt[b][:, :N], r=[K("kt")], w=[K("ktb")])
                eGv = eG[b][:, :N].rearrange("p (c s) -> p c s", s=CH)
                TT(P, "dve", kg[b][:, :N].rearrange("p (c s) -> p c s", s=CH), kt[b][:, :N].rearrange("p (c s) -> p c s", s=CH),
                   eGv[:, :, ce:ce + 1].broadcast_to([128, nch, CH]), ALU.mult, r=[K("kt"), K("eG")], w=[K("kg")])
                for g4 in range(nch // 4):
                    pt = pb[1 + g4 % 2]; ptk = PK(1 + g4 % 2)
                    for c4 in range(4):
                        c = g4 * 4 + c4
                        TR(P, pt[0:CH, c4 * 128:(c4 + 1) * 128], kg[b][:, c * CH:(c + 1) * CH], ident[:], r=[K("kg"), "ident"], w=[ptk])
                    CP(P, "act", kgt[b][:, g4 * 4:(g4 + 1) * 4, :], pt[0:CH, :].rearrange("s (c e) -> s c e", e=128), r=[ptk], w=[K("kgt")])
                for c in range(nch):
                    MM(P, pb[0][0:CH, c * CH:(c + 1) * CH], ktb[b][:, c * CH:(c + 1) * CH], qg[b][:, c * CH:(c + 1) * CH], True, True,
                       r=[K("ktb"), K("qg")], w=[PK(0)])
                TT(P, "dve", atm[b][:, :N], pb[0][0:CH, :N], tri[:, d * 512:d * 512 + N], ALU.mult, r=[PK(0), "tri"], w=[K("atm")])
                po = pb[3 + b]; pok = PK(3 + b)
                for c in (range(nch) if d == 0 else range(nch - 1, -1, -1)):
                    cs = slice(c * CH, (c + 1) * CH)
                    sb_cur = Sb[sbi % 2]; sbk = ("Sb", sbi % 2)
                    MM(P, po[:, cs], v32[b][:, c, :], atm[b][:, cs], True, False, r=[K("v32"), K("atm")], w=[pok])
                    MM(P, po[:, cs], sb_cur[:], qg[b][:, cs], False, True, r=[sbk, K("qg")], w=[pok])
                    pd = pb[5 + pdi % 2]; pdk = PK(5 + pdi % 2); pdi += 1
                    MM(P, pd[:, 0:128], kgt[b][:, c, :], v32[b][:, c, :], True, True, r=[K("kgt"), K("v32")], w=[pdk])
                    STT(P, "dve", S32[:], S32[:], eG[b][:, c * CH + ce:c * CH + ce + 1], pd[:, 0:128], ALU.mult, ALU.add,
                        r=["S32", K("eG"), pdk], w=["S32"])
                    sbi += 1
                    CP(P, "act", Sb[sbi % 2][:], S32[:], r=["S32"], w=[("Sb", sbi % 2)])
                if d == 0:
                    CP(P, "act", ofw[:, t0:t0 + N], po[:, :N], r=[pok], w=[("ofw", ti)])
                else:
                    DMA(P, "sp", sgt[b][:, :N], yT[8, :, t0:t0 + N], w=[K("sg")])
                    TT(P, "dve", osum[b][:, :N], po[:, :N], ofw[:, t0:t0 + N], ALU.add, r=[pok, ("ofw", ti)], w=[K("os")])
                    ACT(P, osq[b][:, :N], osum[b][:, :N], AF.Square, r=[K("os")], w=[K("sq")])
                    MM(P, pb[7][:, :N], ones32[:], osq[b][:, :N], True, True, r=["ones32", K("sq")], w=[PK(7)])
                    RSQRT(P, rst[b][:, :N], pb[7][:, :N], 1.0 / 128.0, LN_EPS, r=[PK(7)], w=[K("rs")])
                    TT(P, "dve", osum[b][:, :N], osum[b][:, :N], rst[b][:, :N], ALU.mult, r=[K("os"), K("rs")], w=[K("os")])
                    STT(P, "dve", cbuf[:, t0:t0 + N], osum[b][:, :N], ng[:, 0:1], sgt[b][:, :N], ALU.mult, ALU.mult,
                        r=[K("os"), "ng", K("sg")], w=[("cbuf", ti)])
        DMA(P, "sp", mixT[384:512, :], cbuf[:], r=[("cbuf", ti) for ti in range(9)])

    with P.scope() as es:
        qb = P.sb("qb", [128, 2, NT], BF16, es=es); kb = P.sb("kb", [128, 2, NT], BF16, es=es)
        vb = P.sb("vb", [128, NT // 128, 256], BF16, es=es)
        cosT = P.sb("cosT", [128, N_LAT], es=es); sinT = P.sb("sinT", [128, N_LAT], es=es)
        rm = P.sb("rm", [128, 128], es=es)
        bbuf = P.sb("bbuf", [128, 2, NT], BF16, es=es)
        dl = P.sb("dl", [128, 512], es=es); lam = P.sb("lam", [128, 4], es=es); lmi = P.sb("lmi", [128, 2], es=es)
        sg2 = P.sb("sg2", [128, 2], es=es)
        DMA(P, "sp", cosT[:], cosd, w=["cosT"]); DMA(P, "sp", sinT[:], sind, w=["sinT"]); DMA(P, "sp", rm[:], rmd, w=["rm"])
        DMA(P, "sp", dl[:], dlam.broadcast_to([128, 512]), w=["dl"]); DMA(P, "sp", lmi[:], lami, w=["lmi"]); DMA(P, "sp", sg2[:], sgl, w=["sg2"])
        for kt4 in range(2):
            h0 = kt4 * 17
            DMA(P, "pool", vb[:, h0:h0 + 17, :], ytok[h0 * 128:(h0 + 17) * 128, 128:384].rearrange("(k s) c -> s k c", s=128), w=["vb"])
        P.op("dve", lambda e: e.memset(lam[:], 0.0), w=["lam"])
        TT(P, "dve", dl[:, 0:128], dl[:, 0:128], dl[:, 128:256], ALU.mult, r=["dl"], w=["dl"])
        TT(P, "dve", dl[:, 256:384], dl[:, 256:384], dl[:, 384:512], ALU.mult, r=["dl"], w=["dl"])
        P.op("dve", lambda e: e.tensor_reduce(out=lam[:, 0:1], in_=dl[:, 0:128], axis=AX.X, op=ALU.add), r=["dl", "lam"], w=["lam"])
        P.op("dve", lambda e: e.tensor_reduce(out=lam[:, 1:2], in_=dl[:, 256:384], axis=AX.X, op=ALU.add), r=["dl", "lam"], w=["lam"])
        ACT(P, lam[:, 0:2], lam[:, 0:2], AF.Exp, r=["lam"], w=["lam"])
        TT(P, "dve", lam[:, 2:3], lam[:, 1:2], lam[:, 0:1], ALU.subtract, r=["lam"], w=["lam"])
        TT(P, "dve", lam[:, 3:4], lam[:, 2:3], lmi[:, 0:1], ALU.subtract, r=["lam", "lmi"], w=["lam"])
        TS(P, "dve", sg2[:], sg2[:], lmi[:, 1:2], None, ALU.mult, None, r=["sg2", "lmi"], w=["sg2"])
        nlam = lam[:, 3:4]
        ld = [P.sb("b_ld", [128, 512], es=es) for _ in range(3)]
        t1 = [P.sb("b_t1", [128, 512], es=es) for _ in range(3)]
        t2 = [P.sb("b_t2", [128, 512], es=es) for _ in range(3)]
        li = 0
        for ti, (t0, N, n) in enumerate(TILES):
            for c in range(4):
                dst = (qb if c < 2 else kb)[:, c % 2, t0:t0 + N]
                dk = ("qk", c, ti)
                b = li % 3; li += 1
                K = lambda s: (s, b)
                DMA(P, "sp", ld[b][:, :N], yT[1 + c, :, t0:t0 + N], w=[K("ld")])
                if n == 1:
                    CP(P, "act", dst, ld[b][:, :N], r=[K("ld")], w=[dk])
                else:
                    l0 = t0 - L_CTX
                    pr = pb[b]; prk = PK(b)
                    MM(P, pr[:, :N], rm[:], ld[b][:, :N], True, True, r=["rm", K("ld")], w=[prk])
                    TT(P, "pool", t1[b][:, :N], ld[b][:, :N], cosT[:, l0:l0 + N], ALU.mult, r=[K("ld"), "cosT"], w=[K("t1")])
                    TT(P, "dve", t2[b][:, :N], pr[:, :N], sinT[:, l0:l0 + N], ALU.mult, r=[prk, "sinT"], w=[K("t2")])
                    TT(P, "pool", dst, t1[b][:, :N], t2[b][:, :N], ALU.add, r=[K("t1"), K("t2")], w=[dk])
        P.barrier()
        pT = [P.sb("b_pT", [128, 512], BF16, es=es) for _ in range(3)]
        rr = P.sb("b_rr", [128, 512], es=es)
        om = [[P.sb("b_om", [128, 512], es=es) for _ in range(2)] for _ in range(2)]
        oo = [P.sb("b_oo", [128, 512], es=es) for _ in range(2)]
        sq = [P.sb("b_sq", [128, 512], es=es) for _ in range(2)]
        rs = P.sb("b_rs", [128, 512], es=es)
        si = 0
        scale = 128.0 ** -0.5
        for ti, (t0, N, n) in enumerate(TILES):
            nkt = 2 if n == 1 else NT // 128
            for m in range(2):
                for kt_ in range(nkt):
                    sp_ = si % 2; pp = si % 3; si += 1
                    MM(P, pb[sp_][:, :N], kb[:, m, kt_ * 128:(kt_ + 1) * 128], qb[:, m, t0:t0 + N], True, True, r=["kb", "qb"], w=[PK(sp_)])
                    ACT(P, pT[pp][:, :N], pb[sp_][:, :N], AF.Exp, r=[PK(sp_)], w=[("pT", pp)], scale=scale)
                    for ec in range(2):
                        MM(P, pb[2 + ec][:, :N], vb[:, kt_, ec * 128:(ec + 1) * 128], pT[pp][:, :N], kt_ == 0, kt_ == nkt - 1,
                           r=["vb", ("pT", pp)], w=[PK(2 + ec)])
                    MM(P, pb[4][:, :N], onesb[:], pT[pp][:, :N], kt_ == 0, kt_ == nkt - 1, r=["onesb", ("pT", pp)], w=[PK(4)])
                P.op("dve", lambda e, N=N: e.reciprocal(out=rr[:, :N], in_=pb[4][:, :N]), r=[PK(4)], w=["rr"])
                for ec in range(2):
                    TT(P, "dve", om[m][ec][:, :N], pb[2 + ec][:, :N], rr[:, :N], ALU.mult, r=[PK(2 + ec), "rr"], w=[("om", m, ec)])
            for ec in range(2):
                STT(P, "dve", oo[ec][:, :N], om[1][ec][:, :N], nlam, om[0][ec][:, :N], ALU.mult, ALU.add,
                    r=[("om", 1, ec), ("om", 0, ec), "lam"], w=[("oo", ec)])
                ACT(P, sq[ec][:, :N], oo[ec][:, :N], AF.Square, r=[("oo", ec)], w=[("sq", ec)])
            for ec in range(2):
                MM(P, pb[5][:, :N], ones32[:], sq[ec][:, :N], ec == 0, ec == 1, r=["ones32", ("sq", ec)], w=[PK(5)])
            RSQRT(P, rs[:, :N], pb[5][:, :N], 1.0 / 256.0, LN_EPS, r=[PK(5)], w=["rs"])
            for ec in range(2):
                TT(P, "dve", oo[ec][:, :N], oo[ec][:, :N], rs[:, :N], ALU.mult, r=[("oo", ec), "rs"], w=[("oo", ec)])
                TS(P, "pool", bbuf[:, ec, t0:t0 + N], oo[ec][:, :N], sg2[:, ec:ec + 1], None, ALU.mult, None, r=[("oo", ec), "sg2"], w=[("bbuf", ec, ti)])
        for ec in range(2):
            DMA(P, "sp", mixT[128 + ec * 128:256 + ec * 128, :], bbuf[:, ec, :], r=[("bbuf", ec, ti) for ti in range(9)])
    P.emit()
    return nc, P


NTF = 1088
NG = 8 * NTF
FT = [(0, 64, 1), (64, 512, 0), (576, 512, 0)]
ALPHA = (2.0 * 4) ** 0.25
NE = 8
FD = 384


def bc_mid(ap2, k):
    n = ap2.shape[1]
    return ap2.rearrange("p (o n) -> p o n", o=1).broadcast_to([128, k, n])


def bc_last(ap2, n):
    k = ap2.shape[1]
    return ap2.rearrange("p (k o) -> p k o", o=1).broadcast_to([128, k, n])


def build_Z():
    nc = bass.Bass("TRN2", target_bir_lowering=False)
    P = Prog(nc)
    cin3 = P.dram("cin3", [128, KC, 3], F32, "ExternalInput")
    wmz = P.dram("wmz", [4 * D, 1536], F32, "ExternalInput")
    bmz = P.dram("bmz", [128, 48], F32, "ExternalInput")
    modz = P.dram("modz", [128, 48, 3], F32, "ExternalOutput")
    pz = P.ps("pz", [128, 512])
    cs = P.sb("cs", [128, KC, 3]); bms = P.sb("bms", [128, 48]); mo = P.sb("mo", [128, 48, 3])
    wbuf = [P.sb("wmb", [128, KC, 512]) for _ in range(2)]
    DMA(P, "sp", cs[:], cin3, w=["cs"])
    DMA(P, "sp", bms[:], bmz, w=["bms"])
    ACT(P, cs[:], cs[:], AF.Silu, r=["cs"], w=["cs"])
    bi = 0
    for l in range(4):
        wv = wmz[l * D:(l + 1) * D, :].rearrange("(k p) c -> p k c", p=128)
        for blk in range(3):
            wb = wbuf[bi % 2]; wk = ("wmb", bi % 2); bi += 1
            for kh in range(2):
                DMA(P, "sp", wb[:, kh * 8:(kh + 1) * 8, :], wv[:, kh * 8:(kh + 1) * 8, blk * 512:(blk + 1) * 512], w=[wk])
            for jj in range(4):
                q = l * 12 + blk * 4 + jj
                for k in range(KC):
                    MM(P, pz[:, 3 * q:3 * q + 3], wb[:, k, jj * 128:(jj + 1) * 128], cs[:, k, :], k == 0, k == KC - 1, r=[wk, "cs"], w=["pz"])
    for n in range(3):
        TT(P, "dve", mo[:, :, n], pz[:, n:144:3], bms[:], ALU.add, r=["pz", "bms"], w=["mo"])
    DMA(P, "sp", modz, mo[:], r=["mo"])
    P.emit()
    return nc, P


def build_F1():
    nc = bass.Bass("TRN2", target_bir_lowering=False)
    P = Prog(nc)
    I = lambda n, s, dt=F32: P.dram(n, s, dt, "ExternalInput")
    xT = I("xT", [D, NTF]); mixT = I("mixT", [D, NTF], BF16); mod2 = I("mod2", [128, 2, 64])
    wout = I("wout", [D, D]); ln1g = I("ln1g", [128, KC]); ln1b = I("ln1b", [128, KC])
    rw = I("rw", [D, 64]); rb = I("rb", [1, 64])
    x1T = P.dram("x1T", [D, NTF], F32, "ExternalOutput")
    hT = P.dram("hT", [D, NTF], BF16, "ExternalOutput")
    comb = P.dram("comb", [NTF, 64], F32, "ExternalOutput")
    pb = [P.ps("pb", [128, 512]) for _ in range(8)]
    PK = lambda i: ("pb", i)
    ones32 = P.sb("ones32", [128, 128])
    P.op("dve", lambda e: e.memset(ones32[:], 1.0), w=["ones32"])
    md = P.sb("md", [128, 2, 64]); lg = P.sb("lg", [128, KC]); lb = P.sb("lb", [128, KC])
    rwt = P.sb("rwt", [128, KC, 64]); rbb = P.sb("rbb", [128, 64])
    wo = P.sb("wo", [128, KC, D], BF16)
    DMA(P, "sp", md[:], mod2, w=["md"]); DMA(P, "sp", lg[:], ln1g, w=["lg"]); DMA(P, "sp", lb[:], ln1b, w=["lb"])
    DMA(P, "sp", rwt[:], rw.rearrange("(k p) e -> p k e", p=128), w=["rwt"])
    DMA(P, "sp", rbb[:], rb.broadcast_to([128, 64]), w=["rbb"])
    wov = wout.rearrange("(k p) c -> p k c", p=128)
    for k4 in range(4):
        DMA(P, "pool", wo[:, k4 * 4:(k4 + 1) * 4, :], wov[:, k4 * 4:(k4 + 1) * 4, :], w=["wo"])
    for n in range(2):
        TS(P, "dve", md[:, n, 32:48], md[:, n, 32:48], 1.0, None, ALU.add, None, r=["md"], w=["md"])
    xt = P.sb("xt", [128, KC, 512]); mx = P.sb("mx", [128, KC, 512], BF16)
    sqt = P.sb("sqt", [128, KC, 512]); hb = P.sb("hb", [128, KC, 512], BF16)
    mean = P.sb("mean", [128, 512]); msq = P.sb("msq", [128, 512]); rstd = P.sb("rstd", [128, 512]); nmr = P.sb("nmr", [128, 512])
    rt = [dict(sc=P.sb("r_sc", [128, 64]), sb=P.sb("r_sb", [128, 64]), t8=P.sb("r_t8", [128, 8]), mk=P.sb("r_mk", [128, 64]),
               dn=P.sb("r_dn", [128, 1]), cm=P.sb("r_cm", [128, 64])) for _ in range(2)]
    xTv = xT.rearrange("(k p) t -> p k t", p=128)
    mTv = mixT.rearrange("(k p) t -> p k t", p=128)
    x1v = x1T.rearrange("(k p) t -> p k t", p=128)
    hTv = hT.rearrange("(k p) t -> p k t", p=128)
    ri = 0
    for ti, (t0, N, n) in enumerate(FT):
        DMA(P, "sp", xt[:, :, :N], xTv[:, :, t0:t0 + N], w=["xt"])
        DMA(P, "sp", mx[:, :, :N], mTv[:, :, t0:t0 + N], w=["mx"])
        TS(P, "pool", xt[:, :, :N], xt[:, :, :N], ALPHA, None, ALU.mult, None, r=["xt"], w=["xt"])
        for dc in range(KC):
            pk = PK(dc % 4)
            for k in range(KC):
                MM(P, pb[dc % 4][:, :N], wo[:, k, dc * 128:(dc + 1) * 128], mx[:, k, :N], k == 0, k == KC - 1, r=["wo", "mx"], w=[pk])
            STT(P, "dve", xt[:, dc, :N], pb[dc % 4][:, :N], md[:, n, dc:dc + 1], xt[:, dc, :N], ALU.mult, ALU.add, r=[pk, "md", "xt"], w=["xt"])
        ACT(P, sqt[:, :, :N], xt[:, :, :N], AF.Square, r=["xt"], w=["sqt"])
        for dc in range(KC):
            MM(P, pb[4][:, :N], ones32[:], xt[:, dc, :N], dc == 0, dc == KC - 1, r=["ones32", "xt"], w=[PK(4)])
        for dc in range(KC):
            MM(P, pb[5][:, :N], ones32[:], sqt[:, dc, :N], dc == 0, dc == KC - 1, r=["ones32", "sqt"], w=[PK(5)])
        P.op("act", lambda e, N=N: e.mul(out=mean[:, :N], in_=pb[4][:, :N], mul=1.0 / D), r=[PK(4)], w=["mean"])
        TT(P, "dve", msq[:, :N], mean[:, :N], mean[:, :N], ALU.mult, r=["mean"], w=["msq"])
        STT(P, "dve", rstd[:, :N], pb[5][:, :N], 1.0 / D, msq[:, :N], ALU.mult, ALU.subtract, r=[PK(5), "msq"], w=["rstd"])
        RSQRT(P, rstd[:, :N], rstd[:, :N], 1.0, LN_EPS, r=["rstd"], w=["rstd"])
        STT(P, "dve", nmr[:, :N], mean[:, :N], -1.0, rstd[:, :N], ALU.mult, ALU.mult, r=["mean", "rstd"], w=["nmr"])
        TT(P, "dve", xt[:, :, :N], xt[:, :, :N], bc_mid(rstd[:, :N], KC), ALU.mult, r=["xt", "rstd"], w=["xt"])
        TT(P, "dve", xt[:, :, :N], xt[:, :, :N], bc_mid(nmr[:, :N], KC), ALU.add, r=["xt", "nmr"], w=["xt"])
        TT(P, "pool", xt[:, :, :N], xt[:, :, :N], bc_last(lg[:, :], N), ALU.mult, r=["xt", "lg"], w=["xt"])
        TT(P, "pool", xt[:, :, :N], xt[:, :, :N], bc_last(lb[:, :], N), ALU.add, r=["xt", "lb"], w=["xt"])
        DMA(P, "sp", x1v[:, :, t0:t0 + N], xt[:, :, :N], r=["xt"], w=[("x1T", ti)])
        TT(P, "dve", sqt[:, :, :N], xt[:, :, :N], bc_last(md[:, n, 32:48], N), ALU.mult, r=["xt", "md"], w=["sqt"])
        TT(P, "pool", sqt[:, :, :N], sqt[:, :, :N], bc_last(md[:, n, 16:32], N), ALU.add, r=["sqt", "md"], w=["sqt"])
        ACT(P, hb[:, :, :N], sqt[:, :, :N], AF.Copy, r=["sqt"], w=["hb"])
        DMA(P, "sp", hTv[:, :, t0:t0 + N], hb[:, :, :N], r=["hb"], w=[("hT", ti)])
        for j in range((N + 127) // 128):
            R = min(128, N - j * 128)
            b = rt[ri % 2]; ri += 1
            K = lambda s: (s, (ri - 1) % 2)
            for k in range(KC):
                MM(P, pb[6][:R, 0:64], sqt[:, k, j * 128:j * 128 + R], rwt[:, k, :], k == 0, k == KC - 1, r=["sqt", "rwt"], w=[PK(6)])
            ACT(P, b["sc"][:R, :], pb[6][:R, 0:64], AF.Sigmoid, r=[PK(6)], w=[K("sc")])
            TT(P, "dve", b["sb"][:R, :], b["sc"][:R, :], rbb[:R, :], ALU.add, r=[K("sc"), "rbb"], w=[K("sb")])
            P.op("dve", lambda e, b=b, R=R: e.max(out=b["t8"][:R, :], in_=b["sb"][:R, :]), r=[K("sb")], w=[K("t8")])
            TS(P, "dve", b["mk"][:R, :], b["sb"][:R, :], b["t8"][:R, 7:8], None, ALU.is_ge, None, r=[K("sb"), K("t8")], w=[K("mk")])
            TT(P, "dve", b["mk"][:R, :], b["mk"][:R, :], b["sc"][:R, :], ALU.mult, r=[K("mk"), K("sc")], w=[K("mk")])
            P.op("dve", lambda e, b=b, R=R: e.tensor_reduce(out=b["dn"][:R, :], in_=b["mk"][:R, :], axis=AX.X, op=ALU.add), r=[K("mk")], w=[K("dn")])
            P.op("dve", lambda e, b=b, R=R: e.reciprocal(out=b["dn"][:R, :], in_=b["dn"][:R, :]), r=[K("dn")], w=[K("dn")])
            TS(P, "dve", b["cm"][:R, :], b["mk"][:R, :], b["dn"][:R, 0:1], 2.5, ALU.mult, ALU.mult, r=[K("mk"), K("dn")], w=[K("cm")])
            DMA(P, "sp", comb[t0 + j * 128:t0 + j * 128 + R, :], b["cm"][:R, :], r=[K("cm")])
    P.emit()
    return nc, P


def build_E():
    nc = bass.Bass("TRN2", target_bir_lowering=False)
    P = Prog(nc)
    I = lambda n, s, dt=F32: P.dram(n, s, dt, "ExternalInput")
    hTa = I("hTa", [D, NG], BF16); hTs = I("hTs", [D, NTF], BF16); cb8 = I("cb8", [NE, NG])
    wg8 = I("wg8", [NE, D, FD]); wu8 = I("wu8", [NE, D, FD]); wd8 = I("wd8", [NE, FD, D])
    sgw = I("sgw", [D, FD]); suw = I("suw", [D, FD]); sdw = I("sdw", [FD, D])
    yp = P.dram("yp", [NG, D], F32, "ExternalOutput")
    ys = P.dram("ys", [NTF, D], F32, "ExternalOutput")
    pb = [P.ps("pb", [128, 512]) for _ in range(8)]
    PK = lambda i: ("pb", i)
    TN = 256
    wg = [P.sb("wg", [128, KC, FD], BF16) for _ in range(4)]
    wu = [P.sb("wu", [128, KC, FD], BF16) for _ in range(4)]
    wd = [P.sb("wd", [128, 3, D], BF16) for _ in range(4)]
    ht = [P.sb("ht", [128, KC, TN], BF16) for _ in range(2)]
    cbt = [P.sb("cbt", [128, TN]) for _ in range(4)]
    sgt = [P.sb("sgt", [128, TN]) for _ in range(2)]
    tmp = [P.sb("tmp", [128, TN]) for _ in range(2)]
    aT = [P.sb("aT", [128, 3, TN], BF16) for _ in range(4)]
    yst = [P.sb("yst", [128, D]) for _ in range(2)]
    prev = P.sb("prev", [128, D])
    cnt = dict(h=0, gu=0, y=0)

    def moe_pass(experts, hsrc, T, out, accumulate, tag):
        ne = len(experts)
        for e, (g_, u_, d_, c_) in enumerate(experts):
            gv = g_.rearrange("(k p) f -> p k f", p=128)
            uv = u_.rearrange("(k p) f -> p k f", p=128)
            for kh in range(2):
                DMA(P, "pool", wg[e][:, kh * 8:(kh + 1) * 8, :], gv[:, kh * 8:(kh + 1) * 8, :], w=[("wg", e)])
                DMA(P, "pool", wu[e][:, kh * 8:(kh + 1) * 8, :], uv[:, kh * 8:(kh + 1) * 8, :], w=[("wu", e)])
            DMA(P, "pool", wd[e][:], d_.rearrange("(c p) d -> p c d", p=128), w=[("wd", e)])
        hv = hsrc.rearrange("(k p) t -> p k t", p=128)
        t0 = 0
        while t0 < T:
            N = min(TN, T - t0)
            hb_ = cnt["h"] % 2; cnt["h"] += 1
            DMA(P, "sp", ht[hb_][:, :, :N], hv[:, :, t0:t0 + N], w=[("ht", hb_)])
            for e, (g_, u_, d_, c_) in enumerate(experts):
                if c_ is not None:
                    DMA(P, "sp", cbt[e][:, :N], c_[:, t0:t0 + N].broadcast_to([128, N]), w=[("cbt", e)])
                for fc in range(3):
                    pi = (cnt["gu"] % 2) * 2; cnt["gu"] += 1
                    sb_ = cnt["gu"] % 2
                    for k in range(KC):
                        MM(P, pb[pi][:, :N], wg[e][:, k, fc * 128:(fc + 1) * 128], ht[hb_][:, k, :N], k == 0, k == KC - 1,
                           r=[("wg", e), ("ht", hb_)], w=[PK(pi)])
                    for k in range(KC):
                        MM(P, pb[pi + 1][:, :N], wu[e][:, k, fc * 128:(fc + 1) * 128], ht[hb_][:, k, :N], k == 0, k == KC - 1,
                           r=[("wu", e), ("ht", hb_)], w=[PK(pi + 1)])
                    ACT(P, sgt[sb_][:, :N], pb[pi][:, :N], AF.Silu, r=[PK(pi)], w=[("sgt", sb_)])
                    if c_ is not None:
                        TT(P, "dve", tmp[sb_][:, :N], sgt[sb_][:, :N], pb[pi + 1][:, :N], ALU.mult, r=[("sgt", sb_), PK(pi + 1)], w=[("tmp", sb_)])
                        TT(P, "pool", aT[e][:, fc, :N], tmp[sb_][:, :N], cbt[e][:, :N], ALU.mult, r=[("tmp", sb_), ("cbt", e)], w=[("aT", e)])
                    else:
                        TT(P, "dve", aT[e][:, fc, :N], sgt[sb_][:, :N], pb[pi + 1][:, :N], ALU.mult, r=[("sgt", sb_), PK(pi + 1)], w=[("aT", e)])
            for j in range((N + 127) // 128):
                R = min(128, N - j * 128)
                r0 = t0 + j * 128
                yb = cnt["y"] % 2; cnt["y"] += 1
                yk = ("yst", yb)
                rowkey = (tag, "rows", r0)
                if accumulate:
                    DMA(P, "sp", prev[:R, :], out[r0:r0 + R, :], r=[("out", r0)], w=["prev"])
                for dq in range(4):
                    first = True
                    for e in range(ne):
                        for fc in range(3):
                            last = (e == ne - 1 and fc == 2)
                            MM(P, pb[4 + dq][:R, :], aT[e][:, fc, j * 128:j * 128 + R], wd[e][:, fc, dq * 512:(dq + 1) * 512], first, last,
                               r=[("aT", e), ("wd", e)], w=[PK(4 + dq)])
                            first = False
                    if accumulate:
                        TT(P, "dve", yst[yb][:R, dq * 512:(dq + 1) * 512], pb[4 + dq][:R, :], prev[:R, dq * 512:(dq + 1) * 512], ALU.add,
                           r=[PK(4 + dq), "prev"], w=[yk])
                    elif dq % 2 == 0:
                        CP(P, "act", yst[yb][:R, dq * 512:(dq + 1) * 512], pb[4 + dq][:R, :], r=[PK(4 + dq)], w=[yk])
                    else:
                        CP(P, "dve", yst[yb][:R, dq * 512:(dq + 1) * 512], pb[4 + dq][:R, :], r=[PK(4 + dq)], w=[yk])
                DMA(P, "sp", out[r0:r0 + R, :], yst[yb][:R, :], r=[yk], w=[("out", r0)])
            t0 += N

    for p in range(2):
        ex = [(wg8[4 * p + e], wu8[4 * p + e], wd8[4 * p + e], cb8[4 * p + e:4 * p + e + 1, :]) for e in range(4)]
        moe_pass(ex, hTa, NG, yp, p == 1, "yp")
    moe_pass([(sgw, suw, sdw, None)], hTs, NTF, ys, False, "ys")
    P.emit()
    return nc, P


def build_F2():
    nc = bass.Bass("TRN2", target_bir_lowering=False)
    P = Prog(nc)
    I = lambda n, s, dt=F32: P.dram(n, s, dt, "ExternalInput")
    x1 = I("x1", [NTF, D]); yps = I("yps", [9, NTF, D]); g2r = I("g2r", [2, D]); ln2g = I("ln2g", [1, D]); ln2b = I("ln2b", [1, D])
    xo = P.dram("xo", [NTF, D], F32, "ExternalOutput")
    g2b = [P.sb("g2b", [128, D]) for _ in range(2)]
    lg = P.sb("lg", [128, D]); lb = P.sb("lb", [128, D])
    for n in range(2):
        DMA(P, "sp", g2b[n][:], g2r[n:n + 1, :].broadcast_to([128, D]), w=[("g2b", n)])
    DMA(P, "sp", lg[:], ln2g.broadcast_to([128, D]), w=["lg"])
    DMA(P, "sp", lb[:], ln2b.broadcast_to([128, D]), w=["lb"])
    NB = 2
    xt = [P.sb("xt", [128, D]) for _ in range(NB)]
    ypb = [P.sb("ypb", [128, D]) for _ in range(4)]
    acc = [[P.sb("acc", [128, D]) for _ in range(2)] for _ in range(NB)]
    st = [P.sb("st", [128, 4, 6]) for _ in range(NB)]
    mv = [P.sb("mv", [128, 2]) for _ in range(NB)]
    subt = [(0, 64, 1)] + [(64 + 128 * i, 128, 0) for i in range(8)]
    yi = 0
    for si, (r0, R, n) in enumerate(subt):
        b = si % NB
        K = lambda s: (s, b)
        DMA(P, "sp", xt[b][:R, :], x1[r0:r0 + R, :], w=[K("xt")])
        for half in range(2):
            eng = "dve" if half == 0 else "pool"
            ids = [0, 1, 2, 3, 4] if half == 0 else [5, 6, 7, 8]
            DMA(P, "sp", acc[b][half][:R, :], yps[ids[0], r0:r0 + R, :], w=[K(("acc", half))])
            for i in ids[1:]:
                yb = yi % 4; yi += 1
                DMA(P, "sp", ypb[yb][:R, :], yps[i, r0:r0 + R, :], w=[("ypb", yb)])
                TT(P, eng, acc[b][half][:R, :], acc[b][half][:R, :], ypb[yb][:R, :], ALU.add, r=[K(("acc", half)), ("ypb", yb)], w=[K(("acc", half))])
        a0 = acc[b][0]; a1 = acc[b][1]
        TT(P, "dve", a0[:R, :], a0[:R, :], a1[:R, :], ALU.add, r=[K(("acc", 0)), K(("acc", 1))], w=[K(("acc", 0))])
        TT(P, "pool", a0[:R, :], a0[:R, :], g2b[n][:R, :], ALU.mult, r=[K(("acc", 0)), ("g2b", n)], w=[K(("acc", 0))])
        STT(P, "dve", a0[:R, :], xt[b][:R, :], ALPHA, a0[:R, :], ALU.mult, ALU.add, r=[K("xt"), K(("acc", 0))], w=[K(("acc", 0))])
        for c in range(4):
            P.op("dve", lambda e, o=st[b], i=a0, c=c, R=R: e.bn_stats(out=o[:R, c, :], in_=i[:R, c * 512:(c + 1) * 512]), r=[K(("acc", 0))], w=[K("st")])
        P.op("dve", lambda e, o=mv[b], i=st[b], R=R: e.bn_aggr(out=o[:R, :], in_=i[:R, :, :]), r=[K("st")], w=[K("mv")])
        RSQRT(P, mv[b][:R, 1:2], mv[b][:R, 1:2], 1.0, LN_EPS, r=[K("mv")], w=[K("mv")])
        TS(P, "dve", a0[:R, :], a0[:R, :], mv[b][:R, 0:1], mv[b][:R, 1:2], ALU.subtract, ALU.mult, r=[K(("acc", 0)), K("mv")], w=[K(("acc", 0))])
        TT(P, "pool", a0[:R, :], a0[:R, :], lg[:R, :], ALU.mult, r=[K(("acc", 0)), "lg"], w=[K(("acc", 0))])
        TT(P, "pool", a0[:R, :], a0[:R, :], lb[:R, :], ALU.add, r=[K(("acc", 0)), "lb"], w=[K(("acc", 0))])
        DMA(P, "sp", xo[r0:r0 + R, :], a0[:R, :], r=[K(("acc", 0))])
    P.emit()
    return nc, P

import math
import numpy as np

D = 2048
L_CTX = 256
N_LAT = 4096
NT = L_CTX + N_LAT
DEPTH = 4
CH = 32


def fm(v):
    v = np.asarray(v, np.float32)
    return np.ascontiguousarray(v.reshape(-1, 128).T)


def consts_M():
    ident = np.eye(128, dtype=np.float32)
    rm = np.zeros((128, 128), np.float32)
    for j in range(128):
        q = j // 32
        if q % 2 == 0:
            rm[j + 32, j] = -1.0
        else:
            rm[j - 32, j] = 1.0
    t = np.arange(N_LAT)
    row = (t // 64).astype(np.float32)
    col = (t % 64).astype(np.float32)
    inv = (10000.0 ** (-np.arange(0, 64, 2, dtype=np.float32) / 64)).astype(np.float32)
    ar = row[:, None] * inv
    ac = col[:, None] * inv
    cos = np.concatenate([np.cos(ar), np.cos(ar), np.cos(ac), np.cos(ac)], axis=-1).astype(np.float32)
    sin = np.concatenate([np.sin(ar), np.sin(ar), np.sin(ac), np.sin(ac)], axis=-1).astype(np.float32)
    mask32 = np.ones((128, 512), np.float32)
    mask32[:, ::CH] = 0.0
    s = np.arange(CH)[:, None]
    tt = np.arange(512)[None, :] % CH
    tri = np.concatenate([(s <= tt), (s >= tt)], axis=1).astype(np.float32)
    return dict(identd=ident, rmd=rm, cosd=np.ascontiguousarray(cos.T), sind=np.ascontiguousarray(sin.T),
                mask32d=mask32, trid=np.ascontiguousarray(tri))


def win_head(w_in_l, h):
    c = lambda base, width: w_in_l[:, base + h * width: base + (h + 1) * width]
    Au, Av = c(0, 128), c(512, 128)
    Bq, Bk, Bv = c(1024, 256), c(2048, 256), c(3072, 256)
    Cq, Cff, Cfb, Ci, Cg = c(4096, 128), c(4608, 128), c(5120, 128), c(5632, 128), c(6144, 128)
    return np.ascontiguousarray(np.concatenate([Au, Bq, Bk, Cq, Cff, Cfb, Cg, Av, Bv, Ci], axis=1))


def M_inputs(inp, l, xfullT, b, h, consts):
    lam_init = 0.8 - 0.6 * math.exp(-0.3 * l)
    wsel = np.zeros((128, 2, 4), np.float32)
    wsel[:, :, 1:l + 1] = 1.0
    lbraw = np.ascontiguousarray(np.transpose(inp["hgrn_lb"][:, :, h * 128:(h + 1) * 128], (2, 1, 0)))
    d = dict(
        xT=xfullT,
        win=win_head(inp["w_in"][l], h),
        lng=inp["gmlp_ln_g"][l][None, h * 128:(h + 1) * 128].copy(),
        lnb=inp["gmlp_ln_b"][l][None, h * 128:(h + 1) * 128].copy(),
        wsT=np.ascontiguousarray(inp["gmlp_ws"][l, h].T),
        bsr=inp["gmlp_bs"][l, h][None, :].copy(),
        dlam=inp["diff_lam"][l].reshape(1, 512).copy(),
        sgl=fm(inp["diff_subln_g"][l]),
        lami=np.tile(np.array([[lam_init, 1.0 - lam_init]], np.float32), (128, 1)),
        lbraw=lbraw, wsel=wsel,
        normg=inp["hgrn_norm_g"][l].reshape(128, 1).copy(),
    )
    d.update(consts)
    return {k: np.ascontiguousarray(v, dtype=np.float32) for k, v in d.items()}

_PROGS = {}
DEPTH_RUN = None
_DBG = None


def _prog(name):
    if name not in _PROGS:
        _PROGS[name] = dict(Z=build_Z, M=build_M, F1=build_F1, E=build_E, F2=build_F2)[name]()[0]
    return _PROGS[name]


def _run(name, in_maps):
    res = run_bass_kernel_spmd(_prog(name), in_maps, core_ids=list(range(8)))
    return res.results


def _f32(a):
    return np.ascontiguousarray(a, dtype=np.float32)


def kernel(**inp):
    inp = {k: np.asarray(v) for k, v in inp.items()}
    consts = consts_M()
    cin3 = _f32(np.stack([fm(inp["c"][0]), fm(inp["c"][1]), fm(inp["c_ctx"])], axis=-1))
    in_maps = []
    for c8 in range(8):
        cols = slice(c8 * 1536, (c8 + 1) * 1536)
        wmz = np.concatenate([inp["w_mod"][l][:, cols] for l in range(DEPTH)], axis=0)
        bmz = np.concatenate([fm(inp["b_mod"][l][cols]) for l in range(DEPTH)], axis=1)
        in_maps.append(dict(cin3=cin3, wmz=_f32(wmz), bmz=_f32(bmz)))
    rz = _run("Z", in_maps)
    mod_all = np.zeros((DEPTH, 3, 6 * D), np.float32)
    for c8 in range(8):
        mz = np.asarray(rz[c8]["modz"])
        for l in range(DEPTH):
            blk = mz[:, l * 12:(l + 1) * 12, :]
            mod_all[l][:, c8 * 1536:(c8 + 1) * 1536] = np.transpose(blk, (2, 1, 0)).reshape(3, 1536)

    def modfm(l, b, lo, nchunks):
        v = np.stack([mod_all[l][b][lo:lo + nchunks * 128], mod_all[l][2][lo:lo + nchunks * 128]], axis=0)
        return _f32(np.transpose(v.reshape(2, nchunks, 128), (2, 0, 1)))

    xs = [_f32(np.concatenate([inp["ctx"][b], inp["x"][b]], axis=0)) for b in range(2)]
    tok_idx = [np.concatenate([np.arange(qd * 64, (qd + 1) * 64), L_CTX + np.arange(qd * 1024, (qd + 1) * 1024)]) for qd in range(4)]
    for l in range(DEPTH_RUN or DEPTH):
        in_maps = []
        for b in range(2):
            xfT = np.ascontiguousarray(xs[b].T)
            for h in range(4):
                d = M_inputs(inp, l, xfT, b, h, consts)
                d["modin"] = modfm(l, b, 0, 32)
                in_maps.append(d)
        rm_ = _run("M", in_maps)
        mix = []
        for b in range(2):
            m = np.zeros((NT, D), dtype=np.asarray(rm_[0]["mixT"]).dtype)
            for h in range(4):
                mt = np.asarray(rm_[b * 4 + h]["mixT"])
                m[:, h * 128:(h + 1) * 128] = mt[0:128].T
                m[:, 512 + h * 256:512 + (h + 1) * 256] = mt[128:384].T
                m[:, 1536 + h * 128:1536 + (h + 1) * 128] = mt[384:512].T
            mix.append(m)
        in_maps = []
        for b in range(2):
            for qd in range(4):
                idx = tok_idx[qd]
                m2 = np.concatenate([modfm(l, b, 4096, 16), modfm(l, b, 6144, 16), modfm(l, b, 8192, 16), modfm(l, b, 10240, 16)], axis=2)
                in_maps.append(dict(
                    xT=np.ascontiguousarray(xs[b][idx].T), mixT=np.ascontiguousarray(mix[b][idx].T), mod2=_f32(m2),
                    wout=_f32(inp["w_out"][l]), ln1g=fm(inp["ln1_g"][l]), ln1b=fm(inp["ln1_b"][l]),
                    rw=_f32(inp["router_w"][l]), rb=_f32(inp["router_b"][l][None, :])))
        rf = _run("F1", in_maps)
        hTa = np.ascontiguousarray(np.concatenate([np.asarray(rf[c]["hT"]) for c in range(8)], axis=1))
        combT = np.ascontiguousarray(np.concatenate([np.asarray(rf[c]["comb"]) for c in range(8)], axis=0).T)
        in_maps = []
        for c8 in range(8):
            es = slice(8 * c8, 8 * c8 + 8)
            in_maps.append(dict(
                hTa=hTa, hTs=np.ascontiguousarray(np.asarray(rf[c8]["hT"])), cb8=_f32(combT[es]),
                wg8=_f32(inp["exp_w_gate"][l][es]), wu8=_f32(inp["exp_w_up"][l][es]), wd8=_f32(inp["exp_w_down"][l][es]),
                sgw=_f32(inp["sh_w_gate"][l]), suw=_f32(inp["sh_w_up"][l]), sdw=_f32(inp["sh_w_down"][l])))
        re_ = _run("E", in_maps)
        in_maps = []
        for b in range(2):
            for qd in range(4):
                c = b * 4 + qd
                yps = np.stack([np.asarray(re_[k]["yp"])[c * 1088:(c + 1) * 1088] for k in range(8)] + [np.asarray(re_[c]["ys"])], axis=0)
                g2r = np.stack([mod_all[l][b][10240:12288], mod_all[l][2][10240:12288]], axis=0)
                in_maps.append(dict(x1=np.ascontiguousarray(np.asarray(rf[c]["x1T"]).T), yps=_f32(yps), g2r=_f32(g2r),
                                    ln2g=_f32(inp["ln2_g"][l][None, :]), ln2b=_f32(inp["ln2_b"][l][None, :])))
        if _DBG is not None and l == 0:
            x1d = np.zeros((2, NT, D), np.float32); hd = np.zeros((2, NT, D), np.float32); yd = np.zeros((2, NT, D), np.float32)
            ysum = sum(np.asarray(re_[k]["yp"]).astype(np.float64) for k in range(8))
            for b in range(2):
                for qd in range(4):
                    c = b * 4 + qd
                    x1d[b][tok_idx[qd]] = np.asarray(rf[c]["x1T"]).T
                    hd[b][tok_idx[qd]] = np.asarray(rf[c]["hT"]).astype(np.float32).T
                    yd[b][tok_idx[qd]] = ysum[c * 1088:(c + 1) * 1088] + np.asarray(re_[c]["ys"])
            _DBG.update(x1=x1d, h=hd, y=yd)
        r2 = _run("F2", in_maps)
        for b in range(2):
            for qd in range(4):
                xs[b][tok_idx[qd]] = np.asarray(r2[b * 4 + qd]["xo"])
    if _DBG is not None:
        _DBG["xs"] = xs
    return np.stack([xs[0][L_CTX:], xs[1][L_CTX:]], axis=0).astype(np.float32)
```

```python
import numpy as np
from contextlib import ExitStack, contextmanager
import concourse.bass as bass
import concourse.mybir as mybir
from concourse.bass_utils import run_bass_kernel_spmd

F32 = mybir.dt.float32
BF16 = mybir.dt.bfloat16
AF = mybir.ActivationFunctionType
ALU = mybir.AluOpType
AX = mybir.AxisListType

NSLOT = 12


class Prog:
    ENG = ("pe", "dve", "act", "pool", "sp")
    DQ = ("sp", "pool")

    def __init__(self, nc):
        self.nc = nc
        self.ops = []
        self.es = ExitStack()
        self.uid = 0

    def sb(self, name, shape, dt=F32, es=None):
        self.uid += 1
        return (es or self.es).enter_context(self.nc.sbuf_tensor("%s_%d" % (name, self.uid), list(shape), dt))

    def ps(self, name, shape, dt=F32, es=None):
        self.uid += 1
        return (es or self.es).enter_context(self.nc.psum_tensor("%s_%d" % (name, self.uid), list(shape), dt))

    def dram(self, name, shape, dt, kind):
        return self.nc.dram_tensor(name, list(shape), dt, kind=kind).ap()

    @contextmanager
    def scope(self):
        es = ExitStack()
        try:
            yield es
        finally:
            self.barrier()
            es.close()

    def op(self, eng, fn, r=(), w=(), dma=False):
        self.ops.append(dict(eng=eng, fn=fn, r=tuple(r), w=tuple(w), dma=dma))

    def cc(self, kind, alu, groups, pairs):
        self.barrier()
        for in_ap, out_ap in pairs:
            self.ops.append(dict(eng="pool", fn=lambda e, i=in_ap, o=out_ap: e.collective_compute(kind, alu, replica_groups=groups, ins=[i], outs=[o]),
                                 r=(), w=(), dma=False, cc=True))
        self.barrier()

    def barrier(self):
        self.ops.append(dict(eng="barrier", fn=None, r=(), w=(), dma=False))

    def eng_obj(self, e):
        nc = self.nc
        return dict(pe=nc.tensor, dve=nc.vector, act=nc.scalar, pool=nc.gpsimd, sp=nc.sync)[e]

    def emit(self):
        nc = self.nc
        ops = self.ops
        n = len(ops)
        last_w = {}
        readers = {}
        deps = [None] * n
        needed = [False] * n
        last_on = {e: None for e in self.ENG}
        for i, o in enumerate(ops):
            if o["eng"] == "barrier":
                last_w = {}
                readers = {}
                for e in self.ENG:
                    if last_on[e] is not None:
                        needed[last_on[e]] = True
                deps[i] = set()
                continue
            d = set()
            for k in o["r"]:
                if k in last_w:
                    d.add(last_w[k])
            for k in o["w"]:
                if k in last_w:
                    d.add(last_w[k])
                seen = {}
                for rr in readers.get(k, ()):
                    if ops[rr]["dma"] or ops[rr].get("cc"):
                        d.add(rr)
                    else:
                        seen[ops[rr]["eng"]] = rr
                d.update(seen.values())
            d.discard(i)
            if o["eng"] == "pe":
                d = {j for j in d if not (ops[j]["eng"] == "pe" and not ops[j]["dma"])}
            deps[i] = d
            for k in o["r"]:
                readers.setdefault(k, []).append(i)
            for k in o["w"]:
                last_w[k] = i
                readers[k] = []
            if not o["dma"] and not o.get("cc"):
                last_on[o["eng"]] = i
        for i in range(n):
            for j in deps[i]:
                needed[j] = True
        if True:
            for e in self.ENG:
                if last_on[e] is not None:
                    needed[last_on[e]] = True
        csem = {e: self.es.enter_context(nc.semaphore("c_" + e)) for e in self.ENG}
        dsem = {e: [self.es.enter_context(nc.semaphore("d_%s_%d" % (e, s))) for s in range(NSLOT)]
                for e in self.DQ}
        ccount = {e: 0 for e in self.ENG}
        dcount = {e: 0 for e in self.DQ}
        sig = [None] * n
        ccsig = []
        waited = {e: {} for e in self.ENG}

        def wait(e, sem, val):
            key = id(sem)
            if waited[e].get(key, 0) >= val:
                return
            self.eng_obj(e).wait_ge(sem, val)
            waited[e][key] = val

        def wait_all(e):
            for e2 in self.ENG:
                if ccount[e2] > 0:
                    wait(e, csem[e2], ccount[e2])
            for q in self.DQ:
                for s in range(NSLOT):
                    cnt = (dcount[q] - s + NSLOT - 1) // NSLOT if dcount[q] > s else 0
                    if cnt > 0:
                        wait(e, dsem[q][s], 16 * cnt)
            for s, v in ccsig:
                wait(e, s, v)

        for i, o in enumerate(ops):
            e = o["eng"]
            if e == "barrier":
                for e2 in self.ENG:
                    wait_all(e2)
                continue
            eo = self.eng_obj(e)
            for j in sorted(deps[i]):
                s, v = sig[j]
                wait(e, s, v)
            if o.get("cc"):
                if not ccsig:
                    ccsig.append([self.es.enter_context(nc.semaphore("cc_all")), 0])
                ccsig[0][1] += 1
                ins = o["fn"](eo)
                ins.then_inc(ccsig[0][0])
                sig[i] = (ccsig[0][0], ccsig[0][1])
            elif o["dma"]:
                jn = dcount[e]
                dcount[e] += 1
                sem = dsem[e][jn % NSLOT]
                prev = 16 * (jn // NSLOT)
                if prev > 0:
                    wait(e, sem, prev)
                ins = o["fn"](eo)
                ins.then_inc(sem, 16)
                sig[i] = (sem, prev + 16)
            else:
                ins = o["fn"](eo)
                if needed[i]:
                    ccount[e] += 1
                    ins.then_inc(csem[e], 1)
                    sig[i] = (csem[e], ccount[e])
        wait_all("sp")
        self.stats = dict(n_ops=n, ccount=dict(ccount), dcount=dict(dcount))
        self.es.close()


D = 2048
KC = 16
L_CTX = 256
N_LAT = 4096
NT = L_CTX + N_LAT
TILES = [(0, 256, 1)] + [(256 + 512 * i, 512, 0) for i in range(8)]
LN_EPS = 1e-5
CH = 32


def DMA(P, q, out, in_, r=(), w=()):
    P.op(q, lambda e, o=out, i=in_: e.dma_start(out=o, in_=i), r=r, w=w, dma=True)


def MM(P, out, lhsT, rhs, start, stop, r=(), w=()):
    P.op("pe", lambda e, o=out, a=lhsT, b=rhs, s=start, t=stop: e.matmul(o, a, b, start=s, stop=t), r=r, w=w)


def TR(P, out, in_, ident, r=(), w=()):
    P.op("pe", lambda e, o=out, a=in_, b=ident: e.transpose(o, a, b), r=r, w=w)


def ACT(P, out, in_, func, r=(), w=(), **kw):
    P.op("act", lambda e, o=out, i=in_, f=func, k=kw: e.activation(out=o, in_=i, func=f, **k), r=r, w=w)


def TT(P, eng, out, in0, in1, op, r=(), w=()):
    P.op(eng, lambda e, o=out, a=in0, b=in1, p=op: e.tensor_tensor(out=o, in0=a, in1=b, op=p), r=r, w=w)


def TS(P, eng, out, in0, s1, s2, op0, op1, r=(), w=()):
    if s2 is None:
        P.op(eng, lambda e, o=out, a=in0, x=s1, p0=op0: e.tensor_scalar(out=o, in0=a, scalar1=x, scalar2=None, op0=p0), r=r, w=w)
    else:
        P.op(eng, lambda e, o=out, a=in0, x=s1, y=s2, p0=op0, p1=op1: e.tensor_scalar(out=o, in0=a, scalar1=x, scalar2=y, op0=p0, op1=p1), r=r, w=w)


def STT(P, eng, out, in0, scalar, in1, op0, op1, r=(), w=()):
    P.op(eng, lambda e, o=out, a=in0, s=scalar, b=in1, p0=op0, p1=op1: e.scalar_tensor_tensor(out=o, in0=a, scalar=s, in1=b, op0=p0, op1=p1), r=r, w=w)


def CP(P, eng, out, in_, r=(), w=()):
    if eng == "act":
        P.op("act", lambda e, o=out, i=in_: e.copy(out=o, in_=i), r=r, w=w)
    else:
        P.op(eng, lambda e, o=out, i=in_: e.tensor_copy(out=o, in_=i), r=r, w=w)


def RSQRT(P, out, in_, scale, eps, r=(), w=()):
    ACT(P, out, in_, AF.Sqrt, r=r, w=w, scale=scale, bias=eps)
    P.op("dve", lambda e, o=out: e.reciprocal(out=o, in_=o), r=w, w=w)


G8 = [list(range(8))]
G4 = [[0, 1, 2, 3], [4, 5, 6, 7]]


def phase_M(P, pb, ident, ones32, onesb, modL_l, modkey, A):
    PK = lambda i: ("pb", i)
    xT_g = A["xT_g"]; win = A["win"]
    lng = A["lng"]; lnb = A["lnb"]; wsT = A["wsT"]; bsr = A["bsr"]
    dlam = A["dlam"]; sgl = A["sgl"]; lami = A["lami"]
    lbraw = A["lbraw"]; wsel = A["wsel"]; normg = A["normg"]
    rmd = A["rmd"]; cosd = A["cosd"]; sind = A["sind"]; mask32d = A["mask32d"]; trid = A["trid"]
    mixT = A["mixT"]; yT = A["yT"]; ytok = A["ytok"]

    with P.scope() as es:
        mod1 = P.sb("mod1", [128, 2, 32], es=es)
        CP(P, "dve", mod1[:], modL_l[:, :, 0:32], r=[modkey], w=["mod1"])
        for n in range(2):
            TS(P, "dve", mod1[:, n, 16:32], mod1[:, n, 16:32], 1.0, None, ALU.add, None, r=["mod1"], w=["mod1"])
        wi = P.sb("wi", [128, KC, 1664], BF16, es=es)
        winv = win.rearrange("(k p) c -> p k c", p=128)
        for k4 in range(4):
            DMA(P, "pool", wi[:, k4 * 4:(k4 + 1) * 4, :], winv[:, k4 * 4:(k4 + 1) * 4, :], w=["wi"])
        stage = [P.sb("xst", [128, 512], es=es) for _ in range(4)]
        xm = [P.sb("xm", [128, KC, 512], BF16, es=es) for _ in range(2)]
        ost = [P.sb("ost", [128, 512], es=es) for _ in range(4)]
        fm_func = [AF.Gelu, AF.Copy, AF.Copy, AF.Copy, AF.Copy, AF.Silu, AF.Sigmoid, AF.Sigmoid, AF.Silu]
        si = 0
        oi = 0
        pi = 0

        def load_tile(ti):
            nonlocal si
            t0, N, n = TILES[ti]
            xmt = xm[ti % 2]
            xk = ("xm", ti % 2)
            for k in range(KC):
                st = stage[si % 4]; sk = ("xst", si % 4); si += 1
                if n == 1:
                    DMA(P, "sp", st[:, :N].rearrange("p (q t) -> p q t", q=4),
                        xT_g[k * 512:(k + 1) * 512, 0:64].rearrange("(q p) t -> p q t", q=4), r=["xT_g"], w=[sk])
                else:
                    q = (ti - 1) // 2
                    off = 64 + ((ti - 1) % 2) * 512
                    DMA(P, "sp", st[:, :N], xT_g[(k * 4 + q) * 128:(k * 4 + q + 1) * 128, off:off + N], r=["xT_g"], w=[sk])
                TS(P, "dve", xmt[:, k, :N], st[:, :N], mod1[:, n, 16 + k:17 + k], mod1[:, n, k:k + 1],
                   ALU.mult, ALU.add, r=[sk, "mod1"], w=[xk])

        load_tile(0)
        for ti, (t0, N, n) in enumerate(TILES):
            xmt = xm[ti % 2]
            xk = ("xm", ti % 2)
            if ti + 1 < len(TILES):
                load_tile(ti + 1)
            for c in range(9):
                pbk = pb[pi % 4]; pk = PK(pi % 4); pi += 1
                for k in range(KC):
                    MM(P, pbk[:, :N], wi[:, k, c * 128:(c + 1) * 128], xmt[:, k, :N], k == 0, k == KC - 1, r=["wi", xk], w=[pk])
                o = ost[oi % 4]; ok = ("ost", oi % 4); oi += 1
                ACT(P, o[:, :N], pbk[:, :N], fm_func[c], r=[pk], w=[ok])
                DMA(P, "pool", yT[c, :, t0:t0 + N], o[:, :N], r=[ok], w=[("yT", c, ti)])
            for j in range(N // 128):
                pbk = pb[pi % 4]; pk = PK(pi % 4); pi += 1
                for k in range(KC):
                    MM(P, pbk[:, :], xmt[:, k, j * 128:(j + 1) * 128], wi[:, k, 1152:1664], k == 0, k == KC - 1, r=["wi", xk], w=[pk])
                o = ost[oi % 4]; ok = ("ost", oi % 4); oi += 1
                ACT(P, o[:, 0:128], pbk[:, 0:128], AF.Gelu, r=[pk], w=[ok])
                CP(P, "dve", o[:, 128:512], pbk[:, 128:512], r=[pk], w=[ok])
                DMA(P, "pool", ytok[t0 + j * 128:t0 + (j + 1) * 128, :], o[:, :], r=[ok], w=[("ytok", (t0 // 128) + j)])


    with P.scope() as es:
        lng_b = P.sb("lng_b", [128, 128], es=es); lnb_b = P.sb("lnb_b", [128, 128], es=es); bs_b = P.sb("bs_b", [128, 128], es=es)
        wsb = P.sb("wsb", [128, 128], BF16, es=es)
        abuf = P.sb("abuf", [128, NT], BF16, es=es)
        DMA(P, "sp", lng_b[:], lng.broadcast_to([128, 128]), w=["lng_b"])
        DMA(P, "sp", lnb_b[:], lnb.broadcast_to([128, 128]), w=["lnb_b"])
        DMA(P, "sp", bs_b[:], bsr.broadcast_to([128, 128]), w=["bs_b"])
        DMA(P, "pool", wsb[:], wsT, w=["wsb"])
        NB = 3
        vt = [P.sb("g_v", [128, 128], es=es) for _ in range(NB)]
        ut = [P.sb("g_u", [128, 128], es=es) for _ in range(NB)]
        stt = [P.sb("g_st", [128, 6], es=es) for _ in range(NB)]
        mv = [P.sb("g_mv", [128, 2], es=es) for _ in range(NB)]
        vn = [P.sb("g_vn", [128, 128], es=es) for _ in range(NB)]
        vl = [P.sb("g_vl", [128, 128], BF16, es=es) for _ in range(NB)]
        sst = [P.sb("g_s", [128, 128], es=es) for _ in range(NB)]
        for ch in range(NT // 128):
            b = ch % NB
            t0 = ch * 128
            K = lambda s: (s, b)
            DMA(P, "sp", vt[b][:], ytok[t0:t0 + 128, 0:128], w=[K("v")])
            DMA(P, "sp", ut[b][:], yT[0, :, t0:t0 + 128], w=[K("u")])
            P.op("dve", lambda e, o=stt[b], i=vt[b]: e.bn_stats(out=o[:], in_=i[:]), r=[K("v")], w=[K("st")])
            P.op("dve", lambda e, o=mv[b], i=stt[b]: e.bn_aggr(out=o[:], in_=i[:]), r=[K("st")], w=[K("mv")])
            RSQRT(P, mv[b][:, 1:2], mv[b][:, 1:2], 1.0, LN_EPS, r=[K("mv")], w=[K("mv")])
            TS(P, "dve", vn[b][:], vt[b][:], mv[b][:, 0:1], mv[b][:, 1:2], ALU.subtract, ALU.mult, r=[K("v"), K("mv")], w=[K("vn")])
            TT(P, "pool", vn[b][:], vn[b][:], lng_b[:], ALU.mult, r=[K("vn"), "lng_b"], w=[K("vn")])
            TT(P, "pool", vl[b][:], vn[b][:], lnb_b[:], ALU.add, r=[K("vn"), "lnb_b"], w=[K("vl")])
            pk = PK(b)
            MM(P, pb[b][:, 0:128], vl[b][:], wsb[:], True, True, r=[K("vl"), "wsb"], w=[pk])
            TT(P, "dve", sst[b][:], pb[b][:, 0:128], bs_b[:], ALU.add, r=[pk, "bs_b"], w=[K("s")])
            TT(P, "pool", abuf[:, t0:t0 + 128], sst[b][:], ut[b][:], ALU.mult, r=[K("s"), K("u")], w=[("abuf", ch)])
        DMA(P, "sp", mixT[0:128, :], abuf[:], r=[("abuf", ch) for ch in range(NT // 128)])

    with P.scope() as es:
        lbr = P.sb("lbr", [128, 2, 4], es=es); wsl = P.sb("wsl", [128, 2, 4], es=es)
        lbv = P.sb("lbv", [128, 2], es=es); oml = P.sb("oml", [128, 2], es=es); den = P.sb("den", [128, 2], es=es)
        ng = P.sb("ng", [128, 1], es=es)
        m32 = P.sb("m32", [128, 512], es=es); tri = P.sb("tri", [CH, 1024], es=es)
        DMA(P, "sp", lbr[:], lbraw, w=["lbr"]); DMA(P, "sp", wsl[:], wsel, w=["wsl"]); DMA(P, "sp", ng[:], normg, w=["ng"])
        DMA(P, "sp", m32[:], mask32d, w=["m32"]); DMA(P, "sp", tri[:], trid, w=["tri"])
        ACT(P, lbr[:], lbr[:], AF.Exp, r=["lbr"], w=["lbr"])
        P.op("dve", lambda e: e.tensor_reduce(out=den[:], in_=lbr[:], axis=AX.X, op=ALU.add), r=["lbr"], w=["den"])
        P.op("dve", lambda e: e.reciprocal(out=den[:], in_=den[:]), r=["den"], w=["den"])
        TT(P, "dve", lbr[:], lbr[:], wsl[:], ALU.mult, r=["lbr", "wsl"], w=["lbr"])
        P.op("dve", lambda e: e.tensor_reduce(out=lbv[:], in_=lbr[:], axis=AX.X, op=ALU.add), r=["lbr"], w=["lbv"])
        TT(P, "dve", lbv[:], lbv[:], den[:], ALU.mult, r=["lbv", "den"], w=["lbv"])
        TS(P, "dve", oml[:], lbv[:], -1.0, 1.0, ALU.mult, ALU.add, r=["lbv"], w=["oml"])

        ofw = P.sb("ofw", [128, NT], es=es)
        cbuf = P.sb("cbuf", [128, NT], BF16, es=es)
        S32 = P.sb("S32", [128, 128], es=es)
        Sb = [P.sb("Sb", [128, 128], BF16, es=es) for _ in range(2)]
        NB = 2
        sig = [P.sb("h_sig", [128, 512], es=es) for _ in range(NB)]
        qin = [P.sb("h_q", [128, 512], es=es) for _ in range(NB)]
        v32 = [P.sb("h_v", [CH, 16, 128], BF16, es=es) for _ in range(NB)]
        ff = [P.sb("h_f", [128, 512], es=es) for _ in range(NB)]
        lf = [P.sb("h_lf", [128, 512], es=es) for _ in range(NB)]
        kk = [P.sb("h_kk", [128, 512], es=es) for _ in range(NB)]
        G = [P.sb("h_G", [128, 512], es=es) for _ in range(NB)]
        eG = [P.sb("h_eG", [128, 512], es=es) for _ in range(NB)]
        eGn = [P.sb("h_eGn", [128, 512], es=es) for _ in range(NB)]
        qg = [P.sb("h_qg", [128, 512], BF16, es=es) for _ in range(NB)]
        kt = [P.sb("h_kt", [128, 512], es=es) for _ in range(NB)]
        ktb = [P.sb("h_ktb", [128, 512], BF16, es=es) for _ in range(NB)]
        kg = [P.sb("h_kg", [128, 512], es=es) for _ in range(NB)]
        kgt = [P.sb("h_kgt", [CH, 16, 128], BF16, es=es) for _ in range(NB)]
        atm = [P.sb("h_atm", [CH, 512], BF16, es=es) for _ in range(NB)]
        osum = [P.sb("h_os", [128, 512], es=es) for _ in range(NB)]
        osq = [P.sb("h_sq", [128, 512], es=es) for _ in range(NB)]
        rst = [P.sb("h_rs", [128, 512], es=es) for _ in range(NB)]
        sgt = [P.sb("h_sg", [128, 512], es=es) for _ in range(NB)]
        it = 0
        sbi = 0
        pdi = 0
        for d in range(2):
            P.op("dve", lambda e: e.memset(S32[:], 0.0), r=["S32"], w=["S32"])
            P.op("dve", lambda e, s=Sb[sbi % 2]: e.memset(s[:], 0.0), r=[("Sb", sbi % 2)], w=[("Sb", sbi % 2)])
            order = list(range(9)) if d == 0 else [0] + list(range(8, 0, -1))
            rv = lambda ap: ap
            for ti in order:
                t0, N, n = TILES[ti]
                nch = N // CH
                b = it % NB; it += 1
                K = lambda s: (s, b)
                DMA(P, "sp", sig[b][:, :N], yT[6 + d, :, t0:t0 + N], w=[K("sig")])
                DMA(P, "sp", qin[b][:, :N], yT[5, :, t0:t0 + N], w=[K("q")])
                vsrc = ytok[t0:t0 + N, 384:512]
                DMA(P, "pool", v32[b][:, :nch, :], vsrc.rearrange("(c s) e -> s c e", s=CH), w=[K("v32")])
                TS(P, "dve", ff[b][:, :N], rv(sig[b][:, :N]), oml[:, d:d + 1], lbv[:, d:d + 1], ALU.mult, ALU.add, r=[K("sig"), "oml", "lbv"], w=[K("f")])
                ACT(P, lf[b][:, :N], ff[b][:, :N], AF.Ln, r=[K("f")], w=[K("lf")])
                TS(P, "pool", kk[b][:, :N], ff[b][:, :N], -1.0, 1.0, ALU.mult, ALU.add, r=[K("f")], w=[K("kk")])
                if d == 0:
                    P.op("dve", lambda e, o=G[b], m=m32, x=lf[b], N=N: e.tensor_tensor_scan(out=o[:, :N], data0=m[:, :N], data1=x[:, :N], initial=0.0, op0=ALU.mult, op1=ALU.add),
                         r=[K("lf"), "m32"], w=[K("G")])
                else:
                    P.op("dve", lambda e, o=G[b], m=m32, x=lf[b], N=N: e.tensor_tensor_scan(out=o[:, :N][:, ::-1], data0=m[:, :N], data1=x[:, :N][:, ::-1], initial=0.0, op0=ALU.mult, op1=ALU.add),
                         r=[K("lf"), "m32"], w=[K("G")])
                ce = CH - 1 if d == 0 else 0
                ACT(P, eG[b][:, :N], G[b][:, :N], AF.Exp, r=[K("G")], w=[K("eG")])
                ACT(P, eGn[b][:, :N], G[b][:, :N], AF.Exp, r=[K("G")], w=[K("eGn")], scale=-1.0)
                TT(P, "dve", qg[b][:, :N], rv(qin[b][:, :N]), eG[b][:, :N], ALU.mult, r=[K("q"), K("eG")], w=[K("qg")])
                TT(P, "pool", kt[b][:, :N], kk[b][:, :N], eGn[b][:, :N], ALU.mult, r=[K("kk"), K("eGn")], w=[K("kt")])
                CP(P, "pool", ktb[b][:, :N], kt[b][:, :N], r=[K("kt")], w=[K("ktb")])
                eGv = eG[b][:, :N].rearrange("p (c s) -> p c s", s=CH)
                TT(P, "dve", kg[b][:, :N].rearrange("p (c s) -> p c s", s=CH), kt[b][:, :N].rearrange("p (c s) -> p c s", s=CH),
                   eGv[:, :, ce:ce + 1].broadcast_to([128, nch, CH]), ALU.mult, r=[K("kt"), K("eG")], w=[K("kg")])
                for g4 in range(nch // 4):
                    pt = pb[1 + g4 % 2]; ptk = PK(1 + g4 % 2)
                    for c4 in range(4):
                        c = g4 * 4 + c4
                        TR(P, pt[0:CH, c4 * 128:(c4 + 1) * 128], kg[b][:, c * CH:(c + 1) * CH], ident[:], r=[K("kg"), "ident"], w=[ptk])
                    CP(P, "act", kgt[b][:, g4 * 4:(g4 + 1) * 4, :], pt[0:CH, :].rearrange("s (c e) -> s c e", e=128), r=[ptk], w=[K("kgt")])
                for c in range(nch):
                    MM(P, pb[0][0:CH, c * CH:(c + 1) * CH], ktb[b][:, c * CH:(c + 1) * CH], qg[b][:, c * CH:(c + 1) * CH], True, True,
                       r=[K("ktb"), K("qg")], w=[PK(0)])
                TT(P, "dve", atm[b][:, :N], pb[0][0:CH, :N], tri[:, d * 512:d * 512 + N], ALU.mult, r=[PK(0), "tri"], w=[K("atm")])
                po = pb[3 + b]; pok = PK(3 + b)
                for c in (range(nch) if d == 0 else range(nch - 1, -1, -1)):
                    cs = slice(c * CH, (c + 1) * CH)
                    sb_cur = Sb[sbi % 2]; sbk = ("Sb", sbi % 2)
                    MM(P, po[:, cs], v32[b][:, c, :], atm[b][:, cs], True, False, r=[K("v32"), K("atm")], w=[pok])
                    MM(P, po[:, cs], sb_cur[:], qg[b][:, cs], False, True, r=[sbk, K("qg")], w=[pok])
                    pd = pb[5 + pdi % 2]; pdk = PK(5 + pdi % 2); pdi += 1
                    MM(P, pd[:, 0:128], kgt[b][:, c, :], v32[b][:, c, :], True, True, r=[K("kgt"), K("v32")], w=[pdk])
                    STT(P, "dve", S32[:], S32[:], eG[b][:, c * CH + ce:c * CH + ce + 1], pd[:, 0:128], ALU.mult, ALU.add,
                        r=["S32", K("eG"), pdk], w=["S32"])
                    sbi += 1
                    CP(P, "act", Sb[sbi % 2][:], S32[:], r=["S32"], w=[("Sb", sbi % 2)])
                if d == 0:
                    CP(P, "act", ofw[:, t0:t0 + N], po[:, :N], r=[pok], w=[("ofw", ti)])
                else:
                    DMA(P, "sp", sgt[b][:, :N], yT[8, :, t0:t0 + N], w=[K("sg")])
                    TT(P, "dve", osum[b][:, :N], po[:, :N], ofw[:, t0:t0 + N], ALU.add, r=[pok, ("ofw", ti)], w=[K("os")])
                    ACT(P, osq[b][:, :N], osum[b][:, :N], AF.Square, r=[K("os")], w=[K("sq")])
                    MM(P, pb[7][:, :N], ones32[:], osq[b][:, :N], True, True, r=["ones32", K("sq")], w=[PK(7)])
                    RSQRT(P, rst[b][:, :N], pb[7][:, :N], 1.0 / 128.0, LN_EPS, r=[PK(7)], w=[K("rs")])
                    TT(P, "dve", osum[b][:, :N], osum[b][:, :N], rst[b][:, :N], ALU.mult, r=[K("os"), K("rs")], w=[K("os")])
                    STT(P, "dve", cbuf[:, t0:t0 + N], osum[b][:, :N], ng[:, 0:1], sgt[b][:, :N], ALU.mult, ALU.mult,
                        r=[K("os"), "ng", K("sg")], w=[("cbuf", ti)])
        DMA(P, "sp", mixT[384:512, :], cbuf[:], r=[("cbuf", ti) for ti in range(9)])

    with P.scope() as es:
        qb = P.sb("qb", [128, 2, NT], BF16, es=es); kb = P.sb("kb", [128, 2, NT], BF16, es=es)
        vb = P.sb("vb", [128, NT // 128, 256], BF16, es=es)
        cosT = P.sb("cosT", [128, N_LAT], es=es); sinT = P.sb("sinT", [128, N_LAT], es=es)
        rm = P.sb("rm", [128, 128], es=es)
        bbuf = P.sb("bbuf", [128, 2, NT], BF16, es=es)
        dl = P.sb("dl", [128, 512], es=es); lam = P.sb("lam", [128, 4], es=es); lmi = P.sb("lmi", [128, 2], es=es)
        sg2 = P.sb("sg2", [128, 2], es=es)
        DMA(P, "sp", cosT[:], cosd, w=["cosT"]); DMA(P, "sp", sinT[:], sind, w=["sinT"]); DMA(P, "sp", rm[:], rmd, w=["rm"])
        DMA(P, "sp", dl[:], dlam.broadcast_to([128, 512]), w=["dl"]); DMA(P, "sp", lmi[:], lami, w=["lmi"]); DMA(P, "sp", sg2[:], sgl, w=["sg2"])
        for kt4 in range(2):
            h0 = kt4 * 17
            DMA(P, "pool", vb[:, h0:h0 + 17, :], ytok[h0 * 128:(h0 + 17) * 128, 128:384].rearrange("(k s) c -> s k c", s=128), w=["vb"])
        P.op("dve", lambda e: e.memset(lam[:], 0.0), w=["lam"])
        TT(P, "dve", dl[:, 0:128], dl[:, 0:128], dl[:, 128:256], ALU.mult, r=["dl"], w=["dl"])
        TT(P, "dve", dl[:, 256:384], dl[:, 256:384], dl[:, 384:512], ALU.mult, r=["dl"], w=["dl"])
        P.op("dve", lambda e: e.tensor_reduce(out=lam[:, 0:1], in_=dl[:, 0:128], axis=AX.X, op=ALU.add), r=["dl", "lam"], w=["lam"])
        P.op("dve", lambda e: e.tensor_reduce(out=lam[:, 1:2], in_=dl[:, 256:384], axis=AX.X, op=ALU.add), r=["dl", "lam"], w=["lam"])
        ACT(P, lam[:, 0:2], lam[:, 0:2], AF.Exp, r=["lam"], w=["lam"])
        TT(P, "dve", lam[:, 2:3], lam[:, 1:2], lam[:, 0:1], ALU.subtract, r=["lam"], w=["lam"])
        TT(P, "dve", lam[:, 3:4], lam[:, 2:3], lmi[:, 0:1], ALU.subtract, r=["lam", "lmi"], w=["lam"])
        TS(P, "dve", sg2[:], sg2[:], lmi[:, 1:2], None, ALU.mult, None, r=["sg2", "lmi"], w=["sg2"])
        nlam = lam[:, 3:4]
        ld = [P.sb("b_ld", [128, 512], es=es) for _ in range(3)]
        t1 = [P.sb("b_t1", [128, 512], es=es) for _ in range(3)]
        t2 = [P.sb("b_t2", [128, 512], es=es) for _ in range(3)]
        li = 0
        for ti, (t0, N, n) in enumerate(TILES):
            for c in range(4):
                dst = (qb if c < 2 else kb)[:, c % 2, t0:t0 + N]
                dk = ("qk", c, ti)
                b = li % 3; li += 1
                K = lambda s: (s, b)
                DMA(P, "sp", ld[b][:, :N], yT[1 + c, :, t0:t0 + N], w=[K("ld")])
                if n == 1:
                    CP(P, "act", dst, ld[b][:, :N], r=[K("ld")], w=[dk])
                else:
                    l0 = t0 - L_CTX
                    pr = pb[b]; prk = PK(b)
                    MM(P, pr[:, :N], rm[:], ld[b][:, :N], True, True, r=["rm", K("ld")], w=[prk])
                    TT(P, "pool", t1[b][:, :N], ld[b][:, :N], cosT[:, l0:l0 + N], ALU.mult, r=[K("ld"), "cosT"], w=[K("t1")])
                    TT(P, "dve", t2[b][:, :N], pr[:, :N], sinT[:, l0:l0 + N], ALU.mult, r=[prk, "sinT"], w=[K("t2")])
                    TT(P, "pool", dst, t1[b][:, :N], t2[b][:, :N], ALU.add, r=[K("t1"), K("t2")], w=[dk])
        P.barrier()
        pT = [P.sb("b_pT", [128, 512], BF16, es=es) for _ in range(3)]
        rr = P.sb("b_rr", [128, 512], es=es)
        om = [[P.sb("b_om", [128, 512], es=es) for _ in range(2)] for _ in range(2)]
        oo = [P.sb("b_oo", [128, 512], es=es) for _ in range(2)]
        sq = [P.sb("b_sq", [128, 512], es=es) for _ in range(2)]
        rs = P.sb("b_rs", [128, 512], es=es)
        si = 0
        scale = 128.0 ** -0.5
        for ti, (t0, N, n) in enumerate(TILES):
            nkt = 2 if n == 1 else NT // 128
            for m in range(2):
                for kt_ in range(nkt):
                    sp_ = si % 2; pp = si % 3; si += 1
                    MM(P, pb[sp_][:, :N], kb[:, m, kt_ * 128:(kt_ + 1) * 128], qb[:, m, t0:t0 + N], True, True, r=["kb", "qb"], w=[PK(sp_)])
                    ACT(P, pT[pp][:, :N], pb[sp_][:, :N], AF.Exp, r=[PK(sp_)], w=[("pT", pp)], scale=scale)
                    for ec in range(2):
                        MM(P, pb[2 + ec][:, :N], vb[:, kt_, ec * 128:(ec + 1) * 128], pT[pp][:, :N], kt_ == 0, kt_ == nkt - 1,
                           r=["vb", ("pT", pp)], w=[PK(2 + ec)])
                    MM(P, pb[4][:, :N], onesb[:], pT[pp][:, :N], kt_ == 0, kt_ == nkt - 1, r=["onesb", ("pT", pp)], w=[PK(4)])
                P.op("dve", lambda e, N=N: e.reciprocal(out=rr[:, :N], in_=pb[4][:, :N]), r=[PK(4)], w=["rr"])
                for ec in range(2):
                    TT(P, "dve", om[m][ec][:, :N], pb[2 + ec][:, :N], rr[:, :N], ALU.mult, r=[PK(2 + ec), "rr"], w=[("om", m, ec)])
            for ec in range(2):
                STT(P, "dve", oo[ec][:, :N], om[1][ec][:, :N], nlam, om[0][ec][:, :N], ALU.mult, ALU.add,
                    r=[("om", 1, ec), ("om", 0, ec), "lam"], w=[("oo", ec)])
                ACT(P, sq[ec][:, :N], oo[ec][:, :N], AF.Square, r=[("oo", ec)], w=[("sq", ec)])
            for ec in range(2):
                MM(P, pb[5][:, :N], ones32[:], sq[ec][:, :N], ec == 0, ec == 1, r=["ones32", ("sq", ec)], w=[PK(5)])
            RSQRT(P, rs[:, :N], pb[5][:, :N], 1.0 / 256.0, LN_EPS, r=[PK(5)], w=["rs"])
            for ec in range(2):
                TT(P, "dve", oo[ec][:, :N], oo[ec][:, :N], rs[:, :N], ALU.mult, r=[("oo", ec), "rs"], w=[("oo", ec)])
                TS(P, "pool", bbuf[:, ec, t0:t0 + N], oo[ec][:, :N], sg2[:, ec:ec + 1], None, ALU.mult, None, r=[("oo", ec), "sg2"], w=[("bbuf", ec, ti)])
        for ec in range(2):
            DMA(P, "sp", mixT[128 + ec * 128:256 + ec * 128, :], bbuf[:, ec, :], r=[("bbuf", ec, ti) for ti in range(9)])
NTF = 1088
NG = 8 * NTF
FT = [(0, 64, 1), (64, 512, 0), (576, 512, 0)]
ALPHA = (2.0 * 4) ** 0.25
NE = 8
FD = 384


def bc_mid(ap2, k):
    n = ap2.shape[1]
    return ap2.rearrange("p (o n) -> p o n", o=1).broadcast_to([128, k, n])


def bc_last(ap2, n):
    k = ap2.shape[1]
    return ap2.rearrange("p (k o) -> p k o", o=1).broadcast_to([128, k, n])


def phase_Z(P, pb, cin3, wmz, bmz, modz_loc, nl):
    nq = 12 * nl
    with P.scope() as es:
        pz = pb[0]
        cs = P.sb("cs", [128, KC, 3], es=es); bms = P.sb("bms", [128, nq], es=es); mo = P.sb("mo", [128, nq, 3], es=es)
        wbuf = [P.sb("wmb", [128, KC, 512], es=es) for _ in range(2)]
        DMA(P, "sp", cs[:], cin3, w=["cs"])
        DMA(P, "sp", bms[:], bmz, w=["bms"])
        ACT(P, cs[:], cs[:], AF.Silu, r=["cs"], w=["cs"])
        bi = 0
        for l in range(nl):
            wv = wmz[l * D:(l + 1) * D, :].rearrange("(k p) c -> p k c", p=128)
            for blk in range(3):
                wb = wbuf[bi % 2]; wk = ("wmb", bi % 2); bi += 1
                for kh in range(2):
                    DMA(P, "sp", wb[:, kh * 8:(kh + 1) * 8, :], wv[:, kh * 8:(kh + 1) * 8, blk * 512:(blk + 1) * 512], w=[wk])
                for jj in range(4):
                    q = l * 12 + blk * 4 + jj
                    for k in range(KC):
                        MM(P, pz[:, 3 * q:3 * q + 3], wb[:, k, jj * 128:(jj + 1) * 128], cs[:, k, :], k == 0, k == KC - 1, r=[wk, "cs"], w=["pz"])
        for n in range(3):
            TT(P, "dve", mo[:, :, n], pz[:, n:3 * nq:3], bms[:], ALU.add, r=["pz", "bms"], w=["mo"])
        DMA(P, "sp", modz_loc.rearrange("p (q n) -> p q n", n=3), mo[:], r=["mo"], w=["modz_loc"])


def phase_modsel(P, pb, ident, modz_g, bsel_d, modL, g2row, nl):
    nq = 12 * nl
    PK = lambda i: ("pb", i)
    with P.scope() as es:
        modall = P.sb("modall", [128, 8, 3 * nq], es=es)
        bsel = P.sb("bsel", [128, 2], es=es)
        tmp = P.sb("modtmp", [128, 8, nq], es=es)
        modsel = P.sb("modsel", [128, 8, nq, 2], es=es)
        g2tmp = P.sb("g2tmp", [128, 32], es=es); g2T = P.sb("g2T", [32, 128], es=es)
        DMA(P, "sp", modall[:], modz_g.rearrange("(r p) c -> p r c", p=128), r=["modz_g"], w=["modall"])
        DMA(P, "sp", bsel[:], bsel_d, w=["bsel"])
        ma = modall[:].rearrange("p r (q n) -> p r q n", n=3)
        TS(P, "dve", tmp[:], ma[:, :, :, 0], bsel[:, 0:1], None, ALU.mult, None, r=["modall", "bsel"], w=["modtmp"])
        STT(P, "dve", modsel[:, :, :, 0], ma[:, :, :, 1], bsel[:, 1:2], tmp[:], ALU.mult, ALU.add, r=["modall", "bsel", "modtmp"], w=["modsel"])
        CP(P, "dve", modsel[:, :, :, 1], ma[:, :, :, 2], r=["modall"], w=["modsel"])
        for l in range(nl):
            for n in range(2):
                CP(P, "dve", modL[l][:, n, :].rearrange("p (c j) -> p c j", j=12), modsel[:, :, l * 12:(l + 1) * 12, n],
                   r=["modsel"], w=[("modL", l)])
            CP(P, "dve", g2tmp[:].rearrange("p (n j) -> p n j", n=2), modL[l][:, :, 80:96], r=[("modL", l)], w=["g2tmp"])
            TR(P, pb[1][0:32, 0:128], g2tmp[:], ident[:], r=["g2tmp", "ident"], w=[PK(1)])
            CP(P, "act", g2T[:], pb[1][0:32, 0:128], r=[PK(1)], w=["g2T"])
            DMA(P, "sp", g2row[l].rearrange("n (j p) -> (n j) p", p=128), g2T[:], r=["g2T"], w=[("g2row", l)])


def phase_F1(P, pb, ident, ones32, modL_l, modkey, A):
    PK = lambda i: ("pb", i)
    xT = A["xT_loc"]; mix_g = A["mix_g"]
    wout = A["wout"]; ln1g = A["ln1g"]; ln1b = A["ln1b"]; rw = A["rw"]; rb = A["rb"]
    x1T = A["x1T"]; hT = A["hT"]; combT = A["combT"]
    with P.scope() as es:
        md = P.sb("md", [128, 2, 64], es=es); lg = P.sb("lg", [128, KC], es=es); lb = P.sb("lb", [128, KC], es=es)
        rwt = P.sb("rwt", [128, KC, 64], es=es); rbb = P.sb("rbb", [128, 64], es=es)
        qsel = P.sb("qsel", [128, 4], es=es)
        wo = P.sb("wo", [128, KC, D], BF16, es=es)
        CP(P, "dve", md[:], modL_l[:, :, 32:96], r=[modkey], w=["md"])
        DMA(P, "sp", lg[:], ln1g, w=["lg"]); DMA(P, "sp", lb[:], ln1b, w=["lb"])
        DMA(P, "sp", rwt[:], rw.rearrange("(k p) e -> p k e", p=128), w=["rwt"])
        DMA(P, "sp", rbb[:], rb.broadcast_to([128, 64]), w=["rbb"])
        DMA(P, "sp", qsel[:], A["qsel"], w=["qsel"])
        wov = wout.rearrange("(k p) c -> p k c", p=128)
        for k4 in range(4):
            DMA(P, "pool", wo[:, k4 * 4:(k4 + 1) * 4, :], wov[:, k4 * 4:(k4 + 1) * 4, :], w=["wo"])
        for n in range(2):
            TS(P, "dve", md[:, n, 32:48], md[:, n, 32:48], 1.0, None, ALU.add, None, r=["md"], w=["md"])
        xt = P.sb("xt", [128, KC, 512], es=es); mx = P.sb("mx", [128, KC, 512], BF16, es=es)
        sqt = P.sb("sqt", [128, KC, 512], es=es); hb = P.sb("hb", [128, KC, 512], BF16, es=es)
        stg1 = P.sb("stg1", [128, KC, 512], BF16, es=es)
        stgs = [(hb, "hb"), (stg1, "stg1")]
        mean = P.sb("mean", [128, 512], es=es); msq = P.sb("msq", [128, 512], es=es)
        rstd = P.sb("rstd", [128, 512], es=es); nmr = P.sb("nmr", [128, 512], es=es)
        rt = [dict(sc=P.sb("r_sc", [128, 64], es=es), sb=P.sb("r_sb", [128, 64], es=es), t8=P.sb("r_t8", [128, 8], es=es),
                   mk=P.sb("r_mk", [128, 64], es=es), dn=P.sb("r_dn", [128, 1], es=es), cm=P.sb("r_cm", [128, 64], es=es),
                   cT=P.sb("r_cT", [128, 128], es=es)) for _ in range(2)]
        for i in range(2):
            P.op("dve", lambda e, t=rt[i]["cm"]: e.memset(t[:], 0.0), w=[("cm", i)])
        xTv = xT.rearrange("(k p) t -> p k t", p=128)
        def mrows(c, h):
            return slice((c * 4 + h) * 64, (c * 4 + h + 1) * 64)
        x1v = x1T.rearrange("(k p) t -> p k t", p=128)
        hTv = hT.rearrange("(k p) t -> p k t", p=128)
        ri = 0
        for ti, (t0, N, n) in enumerate(FT):
            DMA(P, "sp", xt[:, :, :N], xTv[:, :, t0:t0 + N], r=["xT_loc"], w=["xt"])
            for j in range(4):
                stg, sk = stgs[j % 2]
                c0 = j * 64 if n == 1 else 256 + j * 1024 + (t0 - 64)
                q = "sp"
                for half in range(2):
                    ps_ = slice(half * 64, (half + 1) * 64)
                    DMA(P, q, stg[ps_, 0:4, :N], mix_g[half * 256:(half + 1) * 256, c0:c0 + N].rearrange("(h p) t -> p h t", h=4), r=["mix_g"], w=[sk])
                    for h in range(4):
                        for ec in range(2):
                            DMA(P, q, stg[ps_, 4 + 2 * h + ec, :N], mix_g[mrows(2 + 2 * ec + half, h), c0:c0 + N], r=["mix_g"], w=[sk])
                    DMA(P, q, stg[ps_, 12:16, :N], mix_g[(6 + half) * 256:(7 + half) * 256, c0:c0 + N].rearrange("(h p) t -> p h t", h=4), r=["mix_g"], w=[sk])
                if j == 0:
                    TS(P, "dve", mx[:, :, :N], stg[:, :, :N], qsel[:, 0:1], None, ALU.mult, None, r=[sk, "qsel"], w=["mx"])
                else:
                    TS(P, "dve", stg[:, :, :N], stg[:, :, :N], qsel[:, j:j + 1], None, ALU.mult, None, r=[sk, "qsel"], w=[sk])
                    TT(P, "dve", mx[:, :, :N], mx[:, :, :N], stg[:, :, :N], ALU.add, r=[sk, "mx"], w=["mx"])
            TS(P, "pool", xt[:, :, :N], xt[:, :, :N], ALPHA, None, ALU.mult, None, r=["xt"], w=["xt"])
            for dc in range(KC):
                pk = PK(dc % 4)
                for k in range(KC):
                    MM(P, pb[dc % 4][:, :N], wo[:, k, dc * 128:(dc + 1) * 128], mx[:, k, :N], k == 0, k == KC - 1, r=["wo", "mx"], w=[pk])
                STT(P, "dve", xt[:, dc, :N], pb[dc % 4][:, :N], md[:, n, dc:dc + 1], xt[:, dc, :N], ALU.mult, ALU.add, r=[pk, "md", "xt"], w=["xt"])
            ACT(P, sqt[:, :, :N], xt[:, :, :N], AF.Square, r=["xt"], w=["sqt"])
            for dc in range(KC):
                MM(P, pb[4][:, :N], ones32[:], xt[:, dc, :N], dc == 0, dc == KC - 1, r=["ones32", "xt"], w=[PK(4)])
            for dc in range(KC):
                MM(P, pb[5][:, :N], ones32[:], sqt[:, dc, :N], dc == 0, dc == KC - 1, r=["ones32", "sqt"], w=[PK(5)])
            P.op("act", lambda e, N=N: e.mul(out=mean[:, :N], in_=pb[4][:, :N], mul=1.0 / D), r=[PK(4)], w=["mean"])
            TT(P, "dve", msq[:, :N], mean[:, :N], mean[:, :N], ALU.mult, r=["mean"], w=["msq"])
            STT(P, "dve", rstd[:, :N], pb[5][:, :N], 1.0 / D, msq[:, :N], ALU.mult, ALU.subtract, r=[PK(5), "msq"], w=["rstd"])
            RSQRT(P, rstd[:, :N], rstd[:, :N], 1.0, LN_EPS, r=["rstd"], w=["rstd"])
            STT(P, "dve", nmr[:, :N], mean[:, :N], -1.0, rstd[:, :N], ALU.mult, ALU.mult, r=["mean", "rstd"], w=["nmr"])
            TT(P, "dve", xt[:, :, :N], xt[:, :, :N], bc_mid(rstd[:, :N], KC), ALU.mult, r=["xt", "rstd"], w=["xt"])
            TT(P, "dve", xt[:, :, :N], xt[:, :, :N], bc_mid(nmr[:, :N], KC), ALU.add, r=["xt", "nmr"], w=["xt"])
            TT(P, "pool", xt[:, :, :N], xt[:, :, :N], bc_last(lg[:, :], N), ALU.mult, r=["xt", "lg"], w=["xt"])
            TT(P, "pool", xt[:, :, :N], xt[:, :, :N], bc_last(lb[:, :], N), ALU.add, r=["xt", "lb"], w=["xt"])
            DMA(P, "sp", x1v[:, :, t0:t0 + N], xt[:, :, :N], r=["xt"], w=[("x1T", ti)])
            TT(P, "dve", sqt[:, :, :N], xt[:, :, :N], bc_last(md[:, n, 32:48], N), ALU.mult, r=["xt", "md"], w=["sqt"])
            TT(P, "pool", sqt[:, :, :N], sqt[:, :, :N], bc_last(md[:, n, 16:32], N), ALU.add, r=["sqt", "md"], w=["sqt"])
            ACT(P, hb[:, :, :N], sqt[:, :, :N], AF.Copy, r=["sqt"], w=["hb"])
            DMA(P, "sp", hTv[:, :, t0:t0 + N], hb[:, :, :N], r=["hb"], w=[("hT", ti)])
            for j in range((N + 127) // 128):
                R = min(128, N - j * 128)
                b = rt[ri % 2]; ri += 1
                K = lambda s: (s, (ri - 1) % 2)
                for k in range(KC):
                    MM(P, pb[6][:R, 0:64], sqt[:, k, j * 128:j * 128 + R], rwt[:, k, :], k == 0, k == KC - 1, r=["sqt", "rwt"], w=[PK(6)])
                ACT(P, b["sc"][:R, :], pb[6][:R, 0:64], AF.Sigmoid, r=[PK(6)], w=[K("sc")])
                TT(P, "dve", b["sb"][:R, :], b["sc"][:R, :], rbb[:R, :], ALU.add, r=[K("sc"), "rbb"], w=[K("sb")])
                P.op("dve", lambda e, b=b, R=R: e.max(out=b["t8"][:R, :], in_=b["sb"][:R, :]), r=[K("sb")], w=[K("t8")])
                TS(P, "dve", b["mk"][:R, :], b["sb"][:R, :], b["t8"][:R, 7:8], None, ALU.is_ge, None, r=[K("sb"), K("t8")], w=[K("mk")])
                TT(P, "dve", b["mk"][:R, :], b["mk"][:R, :], b["sc"][:R, :], ALU.mult, r=[K("mk"), K("sc")], w=[K("mk")])
                P.op("dve", lambda e, b=b, R=R: e.tensor_reduce(out=b["dn"][:R, :], in_=b["mk"][:R, :], axis=AX.X, op=ALU.add), r=[K("mk")], w=[K("dn")])
                P.op("dve", lambda e, b=b, R=R: e.reciprocal(out=b["dn"][:R, :], in_=b["dn"][:R, :]), r=[K("dn")], w=[K("dn")])
                TS(P, "dve", b["cm"][:R, :], b["mk"][:R, :], b["dn"][:R, 0:1], 2.5, ALU.mult, ALU.mult, r=[K("mk"), K("dn")], w=[K("cm")])
                MM(P, pb[7][0:64, 0:128], b["cm"][:, :], ident[:], True, True, r=[K("cm"), "ident"], w=[PK(7)])
                CP(P, "dve", b["cT"][0:64, :R], pb[7][0:64, 0:R], r=[PK(7)], w=[K("cT")])
                DMA(P, "sp", combT[:, t0 + j * 128:t0 + j * 128 + R], b["cT"][0:64, :R], r=[K("cT")], w=[("combT", t0 + j * 128)])


def phase_E(P, pb, A, l):
    PK = lambda i: ("pb", i)
    hT = A["hT"]; combT = A["combT"]; yp = A["yp"]
    Gg = A["Gg"]; Gu = A["Gu"]; Gd = A["Gd"]
    with P.scope() as es:
        TN = 256
        EP = 2
        wg = [P.sb("wg", [128, KC, FD], BF16, es=es) for _ in range(2 * EP)]
        wu = [P.sb("wu", [128, KC, FD], BF16, es=es) for _ in range(2 * EP)]
        wd = [P.sb("wd", [128, 3, D], BF16, es=es) for _ in range(2 * EP)]
        ht = [P.sb("ht", [128, KC, TN], BF16, es=es) for _ in range(2)]
        cbt = [P.sb("cbt", [128, TN], es=es) for _ in range(2 * EP)]
        sgt = [P.sb("sgt", [128, TN], es=es) for _ in range(2)]
        tmp = [P.sb("tmp", [128, TN], es=es) for _ in range(2)]
        aT = [P.sb("aT", [128, 3, TN], BF16, es=es) for _ in range(EP)]
        yst = [P.sb("yst", [128, D], es=es) for _ in range(2)]
        cnt = dict(h=0, gu=0, y=0, c=0)
        passes = [[(None, None, None, EP * p + e) for e in range(EP)] for p in range(64 // EP)]
        passes.append([(A["sgw"], A["suw"], A["sdw"], None)])
        hv = hT.rearrange("(k p) t -> p k t", p=128)

        def load_weights(pi):
            for e, (g_, u_, d_, ei) in enumerate(passes[pi]):
                b = (pi % 2) * EP + e
                if ei is not None:
                    rk, el = divmod(ei, 8)
                    for fb in range(2):
                        r0 = ((el * 2 + fb) * 8 + rk) * 1024
                        DMA(P, "sp", wg[b][:, fb * 8:(fb + 1) * 8, :], Gg[r0:r0 + 1024, :].rearrange("(kk p) f -> p kk f", p=128), r=["Gg"], w=[("wg", b)])
                        DMA(P, "sp", wu[b][:, fb * 8:(fb + 1) * 8, :], Gu[r0:r0 + 1024, :].rearrange("(kk p) f -> p kk f", p=128), r=["Gu"], w=[("wu", b)])
                    for fc in range(3):
                        r0 = ((el * 3 + fc) * 8 + rk) * 128
                        DMA(P, "sp", wd[b][:, fc, :], Gd[r0:r0 + 128, :], r=["Gd"], w=[("wd", b)])
                    continue
                gv = g_.rearrange("(k p) f -> p k f", p=128)
                uv = u_.rearrange("(k p) f -> p k f", p=128)
                for kh in range(2):
                    DMA(P, "pool", wg[b][:, kh * 8:(kh + 1) * 8, :], gv[:, kh * 8:(kh + 1) * 8, :], w=[("wg", b)])
                    DMA(P, "pool", wu[b][:, kh * 8:(kh + 1) * 8, :], uv[:, kh * 8:(kh + 1) * 8, :], w=[("wu", b)])
                DMA(P, "pool", wd[b][:], d_.rearrange("(c p) d -> p c d", p=128), w=[("wd", b)])

        load_weights(0)
        for pi, experts in enumerate(passes):
            if pi + 1 < len(passes):
                load_weights(pi + 1)
            ne = len(experts)
            accumulate = pi > 0
            t0 = 0
            while t0 < NTF:
                N = min(TN, NTF - t0)
                hb_ = cnt["h"] % 2; cnt["h"] += 1
                DMA(P, "sp", ht[hb_][:, :, :N], hv[:, :, t0:t0 + N], r=["hT"], w=[("ht", hb_)])
                for e, (g_, u_, d_, ei) in enumerate(experts):
                    b = (pi % 2) * EP + e
                    if ei is not None:
                        cb_ = cnt["c"] % (2 * EP); cnt["c"] += 1
                        DMA(P, "sp", cbt[cb_][:, :N], combT[ei:ei + 1, t0:t0 + N].broadcast_to([128, N]), r=["combT"], w=[("cbt", cb_)])
                    for fc in range(3):
                        pi_ = (cnt["gu"] % 2) * 2; cnt["gu"] += 1
                        sb_ = cnt["gu"] % 2
                        for k in range(KC):
                            MM(P, pb[pi_][:, :N], wg[b][:, k, fc * 128:(fc + 1) * 128], ht[hb_][:, k, :N], k == 0, k == KC - 1,
                               r=[("wg", b), ("ht", hb_)], w=[PK(pi_)])
                        for k in range(KC):
                            MM(P, pb[pi_ + 1][:, :N], wu[b][:, k, fc * 128:(fc + 1) * 128], ht[hb_][:, k, :N], k == 0, k == KC - 1,
                               r=[("wu", b), ("ht", hb_)], w=[PK(pi_ + 1)])
                        ACT(P, sgt[sb_][:, :N], pb[pi_][:, :N], AF.Silu, r=[PK(pi_)], w=[("sgt", sb_)])
                        if ei is not None:
                            TT(P, "dve", tmp[sb_][:, :N], sgt[sb_][:, :N], pb[pi_ + 1][:, :N], ALU.mult, r=[("sgt", sb_), PK(pi_ + 1)], w=[("tmp", sb_)])
                            TT(P, "pool", aT[e][:, fc, :N], tmp[sb_][:, :N], cbt[cb_][:, :N], ALU.mult, r=[("tmp", sb_), ("cbt", cb_)], w=[("aT", e)])
                        else:
                            TT(P, "dve", aT[e][:, fc, :N], sgt[sb_][:, :N], pb[pi_ + 1][:, :N], ALU.mult, r=[("sgt", sb_), PK(pi_ + 1)], w=[("aT", e)])
                for j in range((N + 127) // 128):
                    R = min(128, N - j * 128)
                    r0 = t0 + j * 128
                    yb = cnt["y"] % 2; cnt["y"] += 1
                    yk = ("yst", yb)
                    if accumulate:
                        DMA(P, "sp", yst[yb][:R, :], yp[r0:r0 + R, :], r=[("yp", r0)], w=[yk])
                    for dq in range(4):
                        first = True
                        for e in range(ne):
                            b = (pi % 2) * EP + e
                            for fc in range(3):
                                last = (e == ne - 1 and fc == 2)
                                MM(P, pb[4 + dq][:R, :], aT[e][:, fc, j * 128:j * 128 + R], wd[b][:, fc, dq * 512:(dq + 1) * 512], first, last,
                                   r=[("aT", e), ("wd", b)], w=[PK(4 + dq)])
                                first = False
                        if accumulate:
                            TT(P, "dve", yst[yb][:R, dq * 512:(dq + 1) * 512], pb[4 + dq][:R, :], yst[yb][:R, dq * 512:(dq + 1) * 512], ALU.add,
                               r=[PK(4 + dq), yk], w=[yk])
                        elif dq % 2 == 0:
                            CP(P, "act", yst[yb][:R, dq * 512:(dq + 1) * 512], pb[4 + dq][:R, :], r=[PK(4 + dq)], w=[yk])
                        else:
                            CP(P, "dve", yst[yb][:R, dq * 512:(dq + 1) * 512], pb[4 + dq][:R, :], r=[PK(4 + dq)], w=[yk])
                    DMA(P, "sp", yp[r0:r0 + R, :], yst[yb][:R, :], r=[yk], w=[("yp", r0)])
                t0 += N


def phase_F2(P, pb, ident, A, last):
    PK = lambda i: ("pb", i)
    x1T = A["x1T"]; yp = A["yp"]; g2r = A["g2r"]; ln2g = A["ln2g"]; ln2b = A["ln2b"]
    xo = A["xo"]; xT_loc = A["xT_loc"]
    with P.scope() as es:
        g2b = [P.sb("g2b", [128, D], es=es) for _ in range(2)]
        lg = P.sb("lg2", [128, D], es=es); lb = P.sb("lb2", [128, D], es=es)
        for n in range(2):
            DMA(P, "sp", g2b[n][:], g2r[n:n + 1, :].broadcast_to([128, D]), r=["g2row"], w=[("g2b", n)])
        DMA(P, "sp", lg[:], ln2g.broadcast_to([128, D]), w=["lg2"])
        DMA(P, "sp", lb[:], ln2b.broadcast_to([128, D]), w=["lb2"])
        NB = 2
        xf = [P.sb("xf", [128, KC, 128], es=es) for _ in range(NB)]
        ya = [P.sb("ya", [128, D], es=es) for _ in range(NB)]
        xof = [P.sb("xof", [128, KC, 128], es=es) for _ in range(NB)]
        st = [P.sb("st", [128, 4, 6], es=es) for _ in range(NB)]
        mv = [P.sb("mv", [128, 2], es=es) for _ in range(NB)]
        subt = [(0, 64, 1)] + [(64 + 128 * i, 128, 0) for i in range(8)]
        x1v = x1T.rearrange("(k p) t -> p k t", p=128)
        xlv = xT_loc.rearrange("(k p) t -> p k t", p=128)
        for si, (r0, R, n) in enumerate(subt):
            b = si % NB
            K = lambda s: (s, b)
            a0 = ya[b]
            DMA(P, "sp", xf[b][:, :, :R], x1v[:, :, r0:r0 + R], r=["x1T"], w=[K("xf")])
            DMA(P, "pool", a0[:R, :], yp[r0:r0 + R, :], r=["yp"], w=[K("ya")])
            for k in range(KC):
                MM(P, pb[k // 4][:R, (k % 4) * 128:(k % 4 + 1) * 128], xf[b][:, k, :R], ident[:], True, True, r=[K("xf"), "ident"], w=[PK(k // 4)])
            TT(P, "pool", a0[:R, :], a0[:R, :], g2b[n][:R, :], ALU.mult, r=[K("ya"), ("g2b", n)], w=[K("ya")])
            for q in range(4):
                STT(P, "dve", a0[:R, q * 512:(q + 1) * 512], pb[q][:R, :], ALPHA, a0[:R, q * 512:(q + 1) * 512], ALU.mult, ALU.add,
                    r=[PK(q), K("ya")], w=[K("ya")])
            for c in range(4):
                P.op("dve", lambda e, o=st[b], i=a0, c=c, R=R: e.bn_stats(out=o[:R, c, :], in_=i[:R, c * 512:(c + 1) * 512]), r=[K("ya")], w=[K("st")])
            P.op("dve", lambda e, o=mv[b], i=st[b], R=R: e.bn_aggr(out=o[:R, :], in_=i[:R, :, :]), r=[K("st")], w=[K("mv")])
            RSQRT(P, mv[b][:R, 1:2], mv[b][:R, 1:2], 1.0, LN_EPS, r=[K("mv")], w=[K("mv")])
            TS(P, "dve", a0[:R, :], a0[:R, :], mv[b][:R, 0:1], mv[b][:R, 1:2], ALU.subtract, ALU.mult, r=[K("ya"), K("mv")], w=[K("ya")])
            TT(P, "pool", a0[:R, :], a0[:R, :], lg[:R, :], ALU.mult, r=[K("ya"), "lg2"], w=[K("ya")])
            TT(P, "pool", a0[:R, :], a0[:R, :], lb[:R, :], ALU.add, r=[K("ya"), "lb2"], w=[K("ya")])
            if last:
                DMA(P, "sp", xo[r0:r0 + R, :], a0[:R, :], r=[K("ya")], w=[("xo", si)])
            else:
                for k in range(KC):
                    MM(P, pb[4 + k // 4][:, (k % 4) * 128:(k % 4) * 128 + R], a0[:R, k * 128:(k + 1) * 128], ident[:R, :R], True, True, r=[K("ya"), "ident"], w=[PK(4 + k // 4)])
                for q in range(4):
                    CP(P, "act" if q % 2 == 0 else "dve", xof[b][:, 4 * q:4 * q + 4, :R],
                       pb[4 + q][:, :].rearrange("p (c t) -> p c t", t=128)[:, :, :R], r=[PK(4 + q)], w=[K("xof")])
                DMA(P, "sp", xlv[:, :, r0:r0 + R], xof[b][:, :, :R], r=[K("xof")], w=[("xT_loc", si)])


def build_fused(nl=4, upto=None, skipE=False):
    nc = bass.Bass("TRN2", target_bir_lowering=False)
    P = Prog(nc)
    T = lambda n, s, dt=F32: P.dram(n, s, dt, "Internal")
    nq = 12 * nl
    spec = dict(
        cin3=[128, KC, 3], wmz=[nl * D, 1536], bmz=[128, nq], bsel=[128, 2], qsel=[128, 4], x0T=[D, NTF],
        win=[nl, D, 1664], lng=[nl, 1, 128], lnb=[nl, 1, 128], wsT=[nl, 128, 128], bsr=[nl, 1, 128],
        dlam=[nl, 1, 512], sgl=[nl, 128, 2], lami=[nl, 128, 2], lbraw=[128, 2, 4], wsel=[nl, 128, 2, 4], normg=[nl, 128, 1],
        identd=[128, 128], rmd=[128, 128], cosd=[128, N_LAT], sind=[128, N_LAT], mask32d=[128, 512], trid=[CH, 1024],
        wout=[nl, D, D], ln1g=[nl, 128, KC], ln1b=[nl, 128, KC], rw=[nl, D, 64], rb=[nl, 1, 64],
        sgw=[nl, D, FD], suw=[nl, D, FD], sdw=[nl, FD, D],
        ln2g=[nl, 1, D], ln2b=[nl, 1, D])
    for l_ in range(nl):
        spec["wgs%d" % l_] = [NE * D, FD]; spec["wus%d" % l_] = [NE * D, FD]; spec["wds%d" % l_] = [NE * FD, D]
    decl = {}

    def IN(name):
        if name not in decl:
            decl[name] = P.dram(name, spec[name], F32, "ExternalInput")
        return decl[name]

    P.inputs = decl
    modz_loc = T("modz_loc", [128, 3 * nq]); modz_g = T("modz_g", [8 * 128, 3 * nq]); g2row = T("g2row", [nl, 2, D])
    xT_loc = T("xT_loc", [D, NTF]); xT_g = T("xT_g", [4 * D, NTF])
    mixT = T("mixT", [512, NT], BF16); mix_g = T("mix_g", [4 * 512, NT], BF16)
    yT = T("yT_s", [9, 128, NT]); ytok = T("ytok_s", [NT, 512])
    x1T = T("x1T", [D, NTF]); hT = T("hT", [D, NTF], BF16)
    combT = T("combT", [64, NTF]); yp = T("yp", [NTF, D])
    Sg = T("Sg", [NE * D, FD], BF16); Su = T("Su", [NE * D, FD], BF16); Sd = T("Sd", [NE * FD, D], BF16)
    Gg = T("Gg", [8 * NE * D, FD], BF16); Gu = T("Gu", [8 * NE * D, FD], BF16); Gd = T("Gd", [8 * NE * FD, D], BF16)

    pb = [P.ps("pb", [128, 512]) for _ in range(8)]
    ident = P.sb("ident", [128, 128]); ones32 = P.sb("ones32", [128, 128]); onesb = P.sb("onesb", [128, 128], BF16)
    modL = [P.sb("modL", [128, 2, 96]) for _ in range(nl)]
    DMA(P, "sp", ident[:], IN("identd"), w=["ident"])
    P.op("dve", lambda e: e.memset(ones32[:], 1.0), w=["ones32"])
    P.op("dve", lambda e: e.memset(onesb[:], 1.0), w=["onesb"])

    def finish(dumps):
        P.barrier()
        for name, src, shape, dt in dumps:
            o = P.dram(name, shape, dt, "ExternalOutput")
            rows = shape[0]
            step = max(1, rows // 8)
            for r0 in range(0, rows, step):
                DMA(P, "sp", o[r0:min(rows, r0 + step)], src[r0:min(rows, r0 + step)], w=[(name, r0)])
        P.emit()
        return nc, P

    phase_Z(P, pb, IN("cin3"), IN("wmz"), IN("bmz"), modz_loc, nl)
    P.cc("AllGather", ALU.bypass, G8, [(modz_loc, modz_g)])
    phase_modsel(P, pb, ident, modz_g, IN("bsel"), modL, g2row, nl)
    if upto == "Z":
        return finish([("d_modg", modz_g, [8 * 128, 3 * nq], F32), ("d_g2row", g2row[0], [2, D], F32)])
    x0T = IN("x0T")
    for k in range(KC):
        DMA(P, "sp", xT_loc[k * 128:(k + 1) * 128, :], x0T[k * 128:(k + 1) * 128, :], w=["xT_loc"])
    for l in range(nl):
        if upto == "X1" and l == 1:
            P.cc("AllGather", ALU.bypass, G4, [(xT_loc[k * 128:(k + 1) * 128, :], xT_g[k * 512:(k + 1) * 512, :]) for k in range(KC)])
            return finish([("d_xg", xT_g, [4 * D, NTF], F32)])
        P.cc("AllGather", ALU.bypass, G4, [(xT_loc[k * 128:(k + 1) * 128, :], xT_g[k * 512:(k + 1) * 512, :]) for k in range(KC)])
        A = dict(xT_g=xT_g, win=IN("win")[l], lng=IN("lng")[l], lnb=IN("lnb")[l], wsT=IN("wsT")[l], bsr=IN("bsr")[l],
                 dlam=IN("dlam")[l], sgl=IN("sgl")[l], lami=IN("lami")[l],
                 lbraw=IN("lbraw"), wsel=IN("wsel")[l], normg=IN("normg")[l], rmd=IN("rmd"), cosd=IN("cosd"), sind=IN("sind"),
                 mask32d=IN("mask32d"), trid=IN("trid"), mixT=mixT, yT=yT, ytok=ytok)
        phase_M(P, pb, ident, ones32, onesb, modL[l], ("modL", l), A)
        P.cc("AllGather", ALU.bypass, G4, [(mixT[c * 64:(c + 1) * 64, :], mix_g[c * 256:(c + 1) * 256, :]) for c in range(8)])
        if upto == "M":
            return finish([("d_mixg", mix_g, [4 * 512, NT], BF16)])
        A = dict(xT_loc=xT_loc, mix_g=mix_g, wout=IN("wout")[l], ln1g=IN("ln1g")[l], ln1b=IN("ln1b")[l], rw=IN("rw")[l], rb=IN("rb")[l],
                 qsel=IN("qsel"), x1T=x1T, hT=hT, combT=combT)
        phase_F1(P, pb, ident, ones32, modL[l], ("modL", l), A)
        P.barrier()
        if upto == "F1":
            return finish([("d_x1T", x1T, [D, NTF], F32), ("d_hT", hT, [D, NTF], BF16), ("d_combT", combT, [64, NTF], F32)])
        if skipE:
            with P.scope() as es:
                zt = P.sb("zt", [128, D], es=es)
                P.op("dve", lambda e, zt=zt: e.memset(zt[:], 0.0), w=["zt"])
                for r0 in range(0, NTF, 128):
                    R = min(128, NTF - r0)
                    DMA(P, "sp", yp[r0:r0 + R, :], zt[:R, :], r=["zt"], w=[("yp", r0)])
        else:
            with P.scope() as es:
                ca = [P.sb("cva", [128, 4, FD], BF16, es=es) for _ in range(4)]
                cd = [P.sb("cvd", [128, D], BF16, es=es) for _ in range(4)]
                ci = 0
                for nm, Sx in (("wgs", Sg), ("wus", Su)):
                    src = IN("%s%d" % (nm, l))
                    for c in range(NE * D // 512):
                        t = ca[ci % 4]; tk = ("cva", ci % 4); ci += 1
                        DMA(P, "pool", t[:], src[c * 512:(c + 1) * 512, :].rearrange("(kk p) f -> p kk f", p=128), w=[tk])
                        DMA(P, "sp", Sx[c * 512:(c + 1) * 512, :].rearrange("(kk p) f -> p kk f", p=128), t[:], r=[tk], w=[(nm, c)])
                src = IN("wds%d" % l)
                for c in range(NE * FD // 128):
                    t = cd[ci % 4]; tk = ("cvd", ci % 4); ci += 1
                    DMA(P, "pool", t[:], src[c * 128:(c + 1) * 128, :], w=[tk])
                    DMA(P, "sp", Sd[c * 128:(c + 1) * 128, :], t[:], r=[tk], w=[("wds", c)])
            pairs = []
            for Sx, Gx, rows in ((Sg, Gg, 1024), (Su, Gu, 1024), (Sd, Gd, 128)):
                for c in range(Sx.shape[0] // rows):
                    pairs.append((Sx[c * rows:(c + 1) * rows, :], Gx[c * rows * 8:(c + 1) * rows * 8, :]))
            P.cc("AllGather", ALU.bypass, G8, pairs)
            A = dict(hT=hT, combT=combT, yp=yp, Gg=Gg, Gu=Gu, Gd=Gd, sgw=IN("sgw")[l], suw=IN("suw")[l], sdw=IN("sdw")[l])
            phase_E(P, pb, A, l)
        if upto == "E":
            return finish([("d_y", yp, [NTF, D], F32)])
        xo = P.dram("xo", [NTF, D], F32, "ExternalOutput") if l == nl - 1 else None
        A = dict(x1T=x1T, yp=yp, g2r=g2row[l], ln2g=IN("ln2g")[l], ln2b=IN("ln2b")[l], xo=xo, xT_loc=xT_loc)
        phase_F2(P, pb, ident, A, l == nl - 1)
    P.emit()
    return nc, P


import math
import numpy as np

D = 2048
L_CTX = 256
N_LAT = 4096
NT = L_CTX + N_LAT
DEPTH = 4
CH = 32


def fm(v):
    v = np.asarray(v, np.float32)
    return np.ascontiguousarray(v.reshape(-1, 128).T)


def consts_M():
    ident = np.eye(128, dtype=np.float32)
    rm = np.zeros((128, 128), np.float32)
    for j in range(128):
        q = j // 32
        if q % 2 == 0:
            rm[j + 32, j] = -1.0
        else:
            rm[j - 32, j] = 1.0
    t = np.arange(N_LAT)
    row = (t // 64).astype(np.float32)
    col = (t % 64).astype(np.float32)
    inv = (10000.0 ** (-np.arange(0, 64, 2, dtype=np.float32) / 64)).astype(np.float32)
    ar = row[:, None] * inv
    ac = col[:, None] * inv
    cos = np.concatenate([np.cos(ar), np.cos(ar), np.cos(ac), np.cos(ac)], axis=-1).astype(np.float32)
    sin = np.concatenate([np.sin(ar), np.sin(ar), np.sin(ac), np.sin(ac)], axis=-1).astype(np.float32)
    mask32 = np.ones((128, 512), np.float32)
    mask32[:, ::CH] = 0.0
    s = np.arange(CH)[:, None]
    tt = np.arange(512)[None, :] % CH
    tri = np.concatenate([(s <= tt), (s >= tt)], axis=1).astype(np.float32)
    return dict(identd=ident, rmd=rm, cosd=np.ascontiguousarray(cos.T), sind=np.ascontiguousarray(sin.T),
                mask32d=mask32, trid=np.ascontiguousarray(tri))


def win_head(w_in_l, h):
    c = lambda base, width: w_in_l[:, base + h * width: base + (h + 1) * width]
    Au, Av = c(0, 128), c(512, 128)
    Bq, Bk, Bv = c(1024, 256), c(2048, 256), c(3072, 256)
    Cq, Cff, Cfb, Ci, Cg = c(4096, 128), c(4608, 128), c(5120, 128), c(5632, 128), c(6144, 128)
    return np.ascontiguousarray(np.concatenate([Au, Bq, Bk, Cq, Cff, Cfb, Cg, Av, Bv, Ci], axis=1))


_PROG = {}
DEPTH_RUN = None
_DBG = None


def _f32(a):
    return np.ascontiguousarray(a, dtype=np.float32)


def core_inputs(inp, r, nl, consts, cin3):
    b, j = divmod(r, 4)
    L = range(nl)
    d = dict(consts)
    cols = slice(r * 1536, (r + 1) * 1536)
    d["cin3"] = cin3
    d["wmz"] = np.concatenate([inp["w_mod"][l][:, cols] for l in L], axis=0)
    d["bmz"] = np.concatenate([fm(inp["b_mod"][l][cols]) for l in L], axis=1)
    bs = np.zeros((128, 2), np.float32); bs[:, b] = 1.0
    qs = np.zeros((128, 4), np.float32); qs[:, j] = 1.0
    d["bsel"] = bs; d["qsel"] = qs
    d["x0T"] = np.concatenate([inp["ctx"][b][j * 64:(j + 1) * 64], inp["x"][b][j * 1024:(j + 1) * 1024]], axis=0).T
    h = j
    d["win"] = np.stack([win_head(inp["w_in"][l], h) for l in L])
    d["lng"] = np.stack([inp["gmlp_ln_g"][l][None, h * 128:(h + 1) * 128] for l in L])
    d["lnb"] = np.stack([inp["gmlp_ln_b"][l][None, h * 128:(h + 1) * 128] for l in L])
    d["wsT"] = np.stack([inp["gmlp_ws"][l, h].T for l in L])
    d["bsr"] = np.stack([inp["gmlp_bs"][l, h][None, :] for l in L])
    d["dlam"] = np.stack([inp["diff_lam"][l].reshape(1, 512) for l in L])
    d["sgl"] = np.stack([fm(inp["diff_subln_g"][l]) for l in L])
    lam_init = [0.8 - 0.6 * math.exp(-0.3 * l) for l in L]
    d["lami"] = np.stack([np.tile(np.array([[li, 1.0 - li]], np.float32), (128, 1)) for li in lam_init])
    d["lbraw"] = np.transpose(inp["hgrn_lb"][:, :, h * 128:(h + 1) * 128], (2, 1, 0))
    wsel = np.zeros((nl, 128, 2, 4), np.float32)
    for l in L:
        wsel[l, :, :, 1:l + 1] = 1.0
    d["wsel"] = wsel
    d["normg"] = np.stack([inp["hgrn_norm_g"][l].reshape(128, 1) for l in L])
    d["wout"] = inp["w_out"][:nl]
    d["ln1g"] = np.stack([fm(inp["ln1_g"][l]) for l in L]); d["ln1b"] = np.stack([fm(inp["ln1_b"][l]) for l in L])
    d["rw"] = inp["router_w"][:nl]; d["rb"] = inp["router_b"][:nl, None, :]
    es = slice(8 * r, 8 * r + 8)
    for l in L:
        d["wgs%d" % l] = inp["exp_w_gate"][l][es].reshape(8 * D, 384)
        d["wus%d" % l] = inp["exp_w_up"][l][es].reshape(8 * D, 384)
        d["wds%d" % l] = inp["exp_w_down"][l][es].reshape(8 * 384, D)
    d["sgw"] = inp["sh_w_gate"][:nl]; d["suw"] = inp["sh_w_up"][:nl]; d["sdw"] = inp["sh_w_down"][:nl]
    d["ln2g"] = inp["ln2_g"][:nl, None, :]; d["ln2b"] = inp["ln2_b"][:nl, None, :]
    return d


def kernel(**inp):
    inp = {k: np.asarray(v) for k, v in inp.items()}
    nl = DEPTH_RUN or DEPTH
    if nl not in _PROG:
        _PROG[nl] = build_fused(nl)[0]
    consts = consts_M()
    cin3 = _f32(np.stack([fm(inp["c"][0]), fm(inp["c"][1]), fm(inp["c_ctx"])], axis=-1))
    in_maps = [{k: _f32(v) for k, v in core_inputs(inp, r, nl, consts, cin3).items()} for r in range(8)]
    res = run_bass_kernel_spmd(_PROG[nl], in_maps, core_ids=list(range(8)))
    if _DBG is not None:
        _DBG["res"] = res.results
    out = np.zeros((2, N_LAT, D), np.float32)
    for r in range(8):
        b, j = divmod(r, 4)
        out[b, j * 1024:(j + 1) * 1024] = np.asarray(res.results[r]["xo"])[64:]
    return out
```
